# Optimizing a Trainium2 kernel written in Bass

```python
import jax, jax.numpy as jnp
from jax import lax
import numpy as np

D_MODEL = 2048
BATCH = 16
SEQ = 256
DEPTH = 2
DEC_BATCH = 8
DEC_SEQ = 1024
PAST_LEN = 512

GRID_W = 64
N_EVEN = (DEPTH + 1) // 2
N_ODD = DEPTH // 2
N_MOD = 6
EPS = 1e-6
MLA_HEADS = 8
Q_LORA = 512
KV_LORA = 512
QK_NOPE = 128
QK_ROPE = 64
V_HEAD = 128
ROPE_BASE = 10000.0
QBLK = 128
CONV_DIM = D_MODEL - MLA_HEADS * V_HEAD
CONV_W = 3
EVEN_IN = Q_LORA + KV_LORA + QK_ROPE + 3 * CONV_DIM
ML_HEADS = 4
ML_DV = D_MODEL // ML_HEADS
ML_DK = ML_DV // 2
ML_IN = 2 * ML_HEADS * ML_DK + 2 * ML_HEADS * ML_DV + 4 * ML_HEADS
CHUNK = 64
FORGET_BIAS = 3.0
D_FF = 7 * D_MODEL // 2
N_EXPERTS = 8
TOP_K = 2

kernel_name = 'hybrid_mla_conv_mlstm_diffusion_step'


def rmsnorm(x, g):
    xf = x.astype(jnp.float32)
    y = xf * lax.rsqrt(jnp.mean(xf * xf, axis=-1, keepdims=True) + EPS)
    return (y * g.astype(jnp.float32)).astype(x.dtype)


def adaln(cond, w, b, dtype):
    m = (cond @ w.astype(jnp.float32) + b.astype(jnp.float32)).astype(dtype)
    return jnp.split(m[:, None, :], N_MOD, axis=-1)


def modulate(x, g, shift, scale):
    return rmsnorm(x, g) * (1 + scale) + shift


def axial_rope(T):
    rows = T // GRID_W
    row = jnp.repeat(jnp.arange(rows, dtype=jnp.float32), GRID_W)
    col = jnp.tile(jnp.arange(GRID_W, dtype=jnp.float32), rows)
    half = QK_ROPE // 2
    inv_freq = ROPE_BASE ** (-jnp.arange(0, half, 2, dtype=jnp.float32) / half)
    ang_r = row[:, None] * inv_freq
    ang_c = col[:, None] * inv_freq
    return (jnp.cos(ang_r), jnp.sin(ang_r), jnp.cos(ang_c), jnp.sin(ang_c))


def rot_half(x, cos, sin):
    x1, x2 = jnp.split(x, 2, axis=-1)
    return jnp.concatenate([x1 * cos - x2 * sin, x2 * cos + x1 * sin], axis=-1)


def axial_rotate(x, rope):
    cr, sr, cc, sc = rope
    extra = x.ndim - 3
    shp = lambda t: t.reshape(t.shape[:1] + (1,) * extra + t.shape[1:])
    xr, xc = jnp.split(x.astype(jnp.float32), 2, axis=-1)
    out = jnp.concatenate([rot_half(xr, shp(cr), shp(sr)), rot_half(xc, shp(cc), shp(sc))], axis=-1)
    return out.astype(x.dtype)


def block_attention(q, k, v):
    B, Tq, H, dq = q.shape
    nb = Tq // QBLK
    scale = dq ** -0.5
    qb = jnp.moveaxis(q.reshape(B, nb, QBLK, H, dq), 1, 0)

    def one_block(qi):
        s = jnp.einsum('bqhd,bkhd->bhqk', qi, k, preferred_element_type=jnp.float32) * scale
        p = jax.nn.softmax(s, axis=-1).astype(v.dtype)
        return jnp.einsum('bhqk,bkhe->bqhe', p, v)

    out = lax.map(one_block, qb)
    return jnp.moveaxis(out, 0, 1).reshape(B, Tq, H, v.shape[-1])


def short_conv(u, w):
    up = jnp.pad(u, ((0, 0), (1, 1), (0, 0)))
    return up[:, :-2] * w[0] + up[:, 1:-1] * w[1] + up[:, 2:] * w[2]


def mla_keys(ckv, kr, w_ukv):
    B, T, _ = ckv.shape
    kv = (ckv @ w_ukv).reshape(B, T, MLA_HEADS, QK_NOPE + V_HEAD)
    k_nope, v = kv[..., :QK_NOPE], kv[..., QK_NOPE:]
    k_rope = jnp.broadcast_to(kr[:, :, None, :], (B, T, MLA_HEADS, QK_ROPE))
    return jnp.concatenate([k_nope, k_rope], axis=-1), v


def even_mixer(h, rope, ctx_ckv, ctx_kr, w_in, g_q, g_kv, w_uq, w_ukv, conv_w, w_o):
    B, T, _ = h.shape
    c1 = Q_LORA
    c2 = c1 + KV_LORA
    c3 = c2 + QK_ROPE
    c4 = c3 + CONV_DIM
    c5 = c4 + CONV_DIM
    cq, ckv, kr, ux, ub, uc = jnp.split(h @ w_in, [c1, c2, c3, c4, c5], axis=-1)
    cq = rmsnorm(cq, g_q)
    ckv = rmsnorm(ckv, g_kv)
    q = (cq @ w_uq).reshape(B, T, MLA_HEADS, QK_NOPE + QK_ROPE)
    q_nope, q_rope = q[..., :QK_NOPE], q[..., QK_NOPE:]
    kr_pos = kr
    if rope is not None:
        q_rope = axial_rotate(q_rope, rope)
        kr_pos = axial_rotate(kr, rope)
    k, v = mla_keys(ckv, kr_pos, w_ukv)
    if ctx_ckv is not None:
        kc, vc = mla_keys(ctx_ckv, ctx_kr, w_ukv)
        k = jnp.concatenate([k, kc], axis=1)
        v = jnp.concatenate([v, vc], axis=1)
    att = block_attention(jnp.concatenate([q_nope, q_rope], axis=-1), k, v).reshape(B, T, MLA_HEADS * V_HEAD)
    conv = ub * short_conv(uc * ux, conv_w)
    out = jnp.concatenate([att, conv], axis=-1) @ w_o
    return out, ckv, kr


def mlstm_chunkwise(q, k, v, logi, logf, C0, n0, m0):
    B, T, H, _ = q.shape
    dv = v.shape[-1]
    nc = T // CHUNK

    def chunks(a):
        a = a.astype(jnp.float32).reshape((B, nc, CHUNK) + a.shape[2:])
        return jnp.swapaxes(jnp.moveaxis(a, 1, 0), 2, 3)

    causal = jnp.tril(jnp.ones((CHUNK, CHUNK), dtype=bool))

    def step(carry, xs):
        C, n, m = carry
        qc, kc, vc, ic, fc = xs
        b = jnp.cumsum(fc, axis=-1)
        dmat = jnp.where(causal, b[..., :, None] - b[..., None, :] + ic[..., None, :], -jnp.inf)
        a = b + m[..., None]
        mrow = jnp.maximum(a, jnp.max(dmat, axis=-1))
        w_intra = jnp.exp(dmat - mrow[..., None])
        w_inter = jnp.exp(a - mrow)
        s = jnp.einsum('bhjd,bhsd->bhjs', qc, kc) * w_intra
        num = w_inter[..., None] * jnp.einsum('bhjd,bhde->bhje', qc, C) + jnp.einsum('bhjs,bhse->bhje', s, vc)
        den = w_inter * jnp.einsum('bhjd,bhd->bhj', qc, n) + jnp.sum(s, axis=-1)
        hc = num / jnp.maximum(jnp.abs(den), jnp.exp(-mrow))[..., None]
        bl = b[..., -1]
        g = bl[..., None] - b + ic
        m_new = jnp.maximum(bl + m, jnp.max(g, axis=-1))
        wg = jnp.exp(g - m_new[..., None])
        decay = jnp.exp(bl + m - m_new)
        C_new = decay[..., None, None] * C + jnp.einsum('bhs,bhsd,bhse->bhde', wg, kc, vc)
        n_new = decay[..., None] * n + jnp.einsum('bhs,bhsd->bhd', wg, kc)
        return (C_new, n_new, m_new), hc

    init = (C0.astype(jnp.float32), n0.astype(jnp.float32), m0.astype(jnp.float32))
    (C, n, m), hs = lax.scan(step, init, (chunks(q), chunks(k), chunks(v), chunks(logi), chunks(logf)))
    hs = jnp.moveaxis(jnp.swapaxes(hs, 2, 3), 0, 1).reshape(B, T, H, dv)
    return hs, C, n, m


def odd_mixer(h, init, w_in, b_gates, g_h, w_o):
    B, T, _ = h.shape
    hk = ML_HEADS * ML_DK
    hv = ML_HEADS * ML_DV
    q, k, v, o, gates = jnp.split(h @ w_in, [hk, 2 * hk, 2 * hk + hv, 2 * hk + 2 * hv], axis=-1)
    q = q.reshape(B, T, ML_HEADS, ML_DK)
    k = k.reshape(B, T, ML_HEADS, ML_DK) * (ML_DK ** -0.5)
    v = v.reshape(B, T, ML_HEADS, ML_DV)
    g = (gates + b_gates).astype(jnp.float32).reshape(B, T, 4, ML_HEADS)
    logi_f, logf_f = g[:, :, 0], jax.nn.log_sigmoid(g[:, :, 1])
    logi_b, logf_b = g[:, :, 2], jax.nn.log_sigmoid(g[:, :, 3])
    if init is None:
        C0 = jnp.zeros((B, 2, ML_HEADS, ML_DK, ML_DV), jnp.float32)
        n0 = jnp.zeros((B, 2, ML_HEADS, ML_DK), jnp.float32)
        m0 = jnp.zeros((B, 2, ML_HEADS), jnp.float32)
    else:
        C0, n0, m0 = init
    h_f, Cf, nf, mf = mlstm_chunkwise(q, k, v, logi_f, logf_f, C0[:, 0], n0[:, 0], m0[:, 0])
    flip = lambda a: jnp.flip(a, axis=1)
    h_b, Cb, nb, mb = mlstm_chunkwise(flip(q), flip(k), flip(v), flip(logi_b), flip(logf_b), C0[:, 1], n0[:, 1], m0[:, 1])
    ht = rmsnorm(h_f + flip(h_b), g_h.reshape(ML_HEADS, ML_DV))
    y = jax.nn.sigmoid(o.astype(jnp.float32)) * ht.reshape(B, T, hv)
    out = y.astype(h.dtype) @ w_o
    return out, jnp.stack([Cf, Cb], axis=1), jnp.stack([nf, nb], axis=1), jnp.stack([mf, mb], axis=1)


def swiglu(h, w_gate, w_up, w_down):
    return (jax.nn.silu(h @ w_gate) * (h @ w_up)) @ w_down


def moe_ffn(h, w_router, w_gate, w_up, w_down):
    logits = (h @ w_router).astype(jnp.float32)
    top_v, top_i = lax.top_k(logits, TOP_K)
    probs = jax.nn.softmax(top_v, axis=-1)
    dense_gate = jnp.sum(jax.nn.one_hot(top_i, N_EXPERTS, dtype=jnp.float32) * probs[..., None], axis=-2).astype(h.dtype)
    out = jnp.zeros_like(h)
    for e in range(N_EXPERTS):
        out = out + dense_gate[..., e:e + 1] * swiglu(h, w_gate[e], w_up[e], w_down[e])
    return out


def setup_inputs(seed: int = 0) -> dict:
    key = jax.random.key(seed)
    ks = jax.random.split(key, 40)
    nrm = lambda i, shape, s: jax.random.normal(ks[i], shape, jnp.float32) * s
    D = D_MODEL
    gate_offset = jnp.repeat(jnp.array([0.0, FORGET_BIAS, 0.0, FORGET_BIAS], jnp.float32), ML_HEADS)
    return {
        'x_prompt': nrm(0, (BATCH, SEQ, D), 1.0),
        'x_sample': nrm(1, (DEC_BATCH, DEC_SEQ, D), 1.0),
        'c': nrm(2, (DEC_BATCH, D), 1.0),
        'cache_ckv': nrm(3, (DEC_BATCH, N_EVEN, PAST_LEN, KV_LORA), 1.0),
        'cache_krope': nrm(4, (DEC_BATCH, N_EVEN, PAST_LEN, QK_ROPE), 1.0),
        'state_C': nrm(5, (DEC_BATCH, N_ODD, 2, ML_HEADS, ML_DK, ML_DV), 0.1),
        'state_n': nrm(6, (DEC_BATCH, N_ODD, 2, ML_HEADS, ML_DK), 0.1),
        'state_m': nrm(7, (DEC_BATCH, N_ODD, 2, ML_HEADS), 0.5),
        'c_ctx': nrm(8, (D,), 1.0),
        'w_mod': nrm(9, (DEPTH, D, N_MOD * D), 0.5 * D ** -0.5),
        'b_mod': nrm(10, (DEPTH, N_MOD * D), 0.02),
        'g_mix': 1.0 + nrm(11, (DEPTH, D), 0.02),
        'g_ffn': 1.0 + nrm(12, (DEPTH, D), 0.02),
        'w_in_a': nrm(13, (N_EVEN, D, EVEN_IN), D ** -0.5),
        'g_q': 1.0 + nrm(14, (N_EVEN, Q_LORA), 0.02),
        'g_kv': 1.0 + nrm(15, (N_EVEN, KV_LORA), 0.02),
        'w_uq': nrm(16, (N_EVEN, Q_LORA, MLA_HEADS * (QK_NOPE + QK_ROPE)), Q_LORA ** -0.5),
        'w_ukv': nrm(17, (N_EVEN, KV_LORA, MLA_HEADS * (QK_NOPE + V_HEAD)), KV_LORA ** -0.5),
        'conv_w': nrm(18, (N_EVEN, CONV_W, CONV_DIM), CONV_W ** -0.5),
        'w_o_a': nrm(19, (N_EVEN, D, D), D ** -0.5),
        'w_in_c': nrm(20, (N_ODD, D, ML_IN), D ** -0.5),
        'b_gates': nrm(21, (N_ODD, 4 * ML_HEADS), 0.1) + gate_offset[None],
        'g_h': 1.0 + nrm(22, (N_ODD, ML_HEADS * ML_DV), 0.02),
        'w_o_c': nrm(23, (N_ODD, D, D), D ** -0.5),
        'w_ffn_gate': nrm(24, (N_EVEN, D, D_FF), D ** -0.5),
        'w_ffn_up': nrm(25, (N_EVEN, D, D_FF), D ** -0.5),
        'w_ffn_down': nrm(26, (N_EVEN, D_FF, D), D_FF ** -0.5),
        'w_router': nrm(27, (N_ODD, D, N_EXPERTS), D ** -0.5),
        'w_exp_gate': nrm(28, (N_ODD, N_EXPERTS, D, D_FF), D ** -0.5),
        'w_exp_up': nrm(29, (N_ODD, N_EXPERTS, D, D_FF), D ** -0.5),
        'w_exp_down': nrm(30, (N_ODD, N_EXPERTS, D_FF, D), D_FF ** -0.5),
        'g_final': 1.0 + nrm(31, (D,), 0.02),
    }


def reference(x_prompt, x_sample, c, cache_ckv, cache_krope, state_C, state_n, state_m,
              c_ctx, w_mod, b_mod, g_mix, g_ffn,
              w_in_a, g_q, g_kv, w_uq, w_ukv, conv_w, w_o_a,
              w_in_c, b_gates, g_h, w_o_c,
              w_ffn_gate, w_ffn_up, w_ffn_down,
              w_router, w_exp_gate, w_exp_up, w_exp_down, g_final):
    rope = axial_rope(x_sample.shape[1])
    cond_p = jax.nn.silu(c_ctx.astype(jnp.float32))[None]
    cond_s = jax.nn.silu(c.astype(jnp.float32))
    xp, xs = x_prompt, x_sample
    ckv_list, kr_list, C_list, n_list, m_list = [], [], [], [], []
    for l in range(DEPTH):
        j = l // 2
        mp = adaln(cond_p, w_mod[l], b_mod[l], xp.dtype)
        ms = adaln(cond_s, w_mod[l], b_mod[l], xs.dtype)
        hp = modulate(xp, g_mix[l], mp[0], mp[1])
        hs = modulate(xs, g_mix[l], ms[0], ms[1])
        if l % 2 == 0:
            op, ckv_p, kr_p = even_mixer(hp, None, None, None, w_in_a[j], g_q[j], g_kv[j], w_uq[j], w_ukv[j], conv_w[j], w_o_a[j])
            os_, _, _ = even_mixer(hs, rope, cache_ckv[:, j], cache_krope[:, j], w_in_a[j], g_q[j], g_kv[j], w_uq[j], w_ukv[j], conv_w[j], w_o_a[j])
            ckv_list.append(ckv_p)
            kr_list.append(kr_p)
        else:
            op, Cp, np_, mp_ = odd_mixer(hp, None, w_in_c[j], b_gates[j], g_h[j], w_o_c[j])
            os_, _, _, _ = odd_mixer(hs, (state_C[:, j], state_n[:, j], state_m[:, j]), w_in_c[j], b_gates[j], g_h[j], w_o_c[j])
            C_list.append(Cp)
            n_list.append(np_)
            m_list.append(mp_)
        xp = xp + mp[2] * op
        xs = xs + ms[2] * os_
        hp = modulate(xp, g_ffn[l], mp[3], mp[4])
        hs = modulate(xs, g_ffn[l], ms[3], ms[4])
        if l % 2 == 0:
            fp = swiglu(hp, w_ffn_gate[j], w_ffn_up[j], w_ffn_down[j])
            fs = swiglu(hs, w_ffn_gate[j], w_ffn_up[j], w_ffn_down[j])
        else:
            fp = moe_ffn(hp, w_router[j], w_exp_gate[j], w_exp_up[j], w_exp_down[j])
            fs = moe_ffn(hs, w_router[j], w_exp_gate[j], w_exp_up[j], w_exp_down[j])
        xp = xp + mp[5] * fp
        xs = xs + ms[5] * fs
    y_prompt = rmsnorm(xp, g_final)
    y_sample = rmsnorm(xs, g_final)
    new_cache_ckv = jnp.stack(ckv_list, axis=1)
    new_cache_krope = jnp.stack(kr_list, axis=1)
    new_state_C = jnp.stack(C_list, axis=1)
    new_state_n = jnp.stack(n_list, axis=1)
    new_state_m = jnp.stack(m_list, axis=1)
    return (y_prompt, y_sample, new_cache_ckv, new_cache_krope, new_state_C, new_state_n, new_state_m)
```

```python
import math
import numpy as np
import concourse.bass as bass
import concourse.mybir as mybir
from concourse.bass_utils import run_bass_kernel_spmd

F32 = mybir.dt.float32
BF16 = mybir.dt.bfloat16
ALU = mybir.AluOpType
AF = mybir.ActivationFunctionType
AX = mybir.AxisListType

D = 2048
NCH = 16
TP = 256
TS = 1024
TT = 1536
DFF = 7168
NFC = DFF // 128
NE = 8
EPS = 1e-6
C1, C2, C3, C4, C5 = 512, 1024, 1088, 2112, 3136
COMPUTE = ('pe', 'act', 'dve', 'pool')


class Buf:
    __slots__ = ('name', 'w', 'r')

    def __init__(self, name=''):
        self.name = name
        self.w = None
        self.r = []


class Ins:
    __slots__ = ('eng', 'fn', 'deps', 'flag', 'ms', 'is_dma', 'sem', 'val', 'line')

    def __init__(self, eng, fn, deps, is_dma=False, sem=None, val=0):
        import sys as _s
        fr = _s._getframe(2)
        ln = []
        while fr is not None and len(ln) < 4:
            ln.append(fr.f_lineno)
            fr = fr.f_back
        self.line = ln
        self.eng = eng
        self.fn = fn
        self.deps = deps
        self.flag = False
        self.ms = 0
        self.is_dma = is_dma
        self.sem = sem
        self.val = val


class DSem:
    def __init__(self, prog, name, serial=False):
        self.h = prog.nc.alloc_semaphore(name=name)
        self.count = 0
        self.serial = serial
        self.last = None


class Prog:
    def __init__(self, nc):
        self.nc = nc
        self.q = {e: [] for e in ('pe', 'act', 'dve', 'pool', 'sp')}
        self.msem = {}
        self.nsem = 0
        self.out_dmas = []
        self.fence = {}
        self.all_dsems = []

    def dsem(self, name=None, serial=False):
        self.nsem += 1
        d = DSem(self, name or f"ds{self.nsem}", serial)
        self.all_dsems.append(d)
        return d

    def barrier(self):
        deps = []
        for e, lst in self.q.items():
            for ins in reversed(lst):
                if not ins.is_dma:
                    deps.append(ins)
                    break
        for d in self.all_dsems:
            if d.last is not None:
                deps.append(d.last)
        for e in self.q:
            self.fence[e] = list(deps)

    def _fence(self, eng, deps):
        f = self.fence.pop(eng, None)
        if f:
            deps.extend(f)
        return deps

    def _deps(self, reads, writes, extra):
        deps = []
        for b in reads:
            if b.w is not None:
                deps.append(b.w)
        for b in writes:
            if b.w is not None:
                deps.append(b.w)
            deps.extend(b.r)
        if extra:
            deps.extend(extra)
        return deps

    def _commit(self, ins, reads, writes):
        for b in reads:
            b.r.append(ins)
            if len(b.r) > 48:
                last = {}
                keep = []
                for i in b.r:
                    if i.is_dma:
                        keep.append(i)
                    else:
                        last[i.eng] = i
                b.r = keep + list(last.values())
        for b in writes:
            b.w = ins
            b.r = []

    def op(self, eng, fn, reads=(), writes=(), extra=None):
        ins = Ins(eng, fn, self._fence(eng, self._deps(reads, writes, extra)))
        self.q[eng].append(ins)
        self._commit(ins, reads, writes)
        return ins

    def dma(self, eng, out, in_, sem, reads=(), writes=(), extra=None, is_output=False, **kw):
        deps = self._fence(eng, self._deps(reads, writes, extra))
        if sem.serial and sem.last is not None:
            deps.append(sem.last)
        sem.count += 16
        ins = Ins(eng, None, deps, is_dma=True, sem=sem, val=sem.count)
        sem.last = ins
        ins.fn = lambda e, out=out, in_=in_, kw=kw: e.dma_start(out=out, in_=in_, **kw)
        self.q[eng].append(ins)
        self._commit(ins, reads, writes)
        if is_output:
            self.out_dmas.append(ins)
        return ins

    def finalize(self):
        nc = self.nc
        for e, lst in self.q.items():
            for ins in lst:
                for d in ins.deps:
                    if not d.is_dma and not (d.eng == 'pe' and ins.eng == 'pe'):
                        d.flag = True
        for e in COMPUTE:
            n = 0
            for ins in self.q[e]:
                if not ins.is_dma and ins.flag:
                    n += 1
                    ins.ms = n
            self.msem[e] = nc.alloc_semaphore(name=f"ms_{e}")
        final = {}
        for d in self.out_dmas:
            k = id(d.sem)
            if k not in final or final[k][1] < d.val:
                final[k] = (d.sem, d.val)
        engmap = {'pe': 'tensor', 'act': 'scalar', 'dve': 'vector', 'pool': 'gpsimd', 'sp': 'sync'}
        stats = {}
        with nc.Block() as block:
            for e in ('sp', 'pool', 'act', 'dve', 'pe'):
                lst = self.q[e]

                def body(eng, e=e, lst=lst):
                    waited = {}
                    nw = 0
                    for ins in lst:
                        need = {}
                        for d in ins.deps:
                            if d.is_dma:
                                key = ('d', id(d.sem))
                                h = d.sem.h
                                v = d.val
                            else:
                                if d.eng == 'pe' and e == 'pe':
                                    continue
                                key = ('m', d.eng)
                                h = self.msem[d.eng]
                                v = d.ms
                            if key not in need or need[key][1] < v:
                                need[key] = (h, v)
                        for key, (h, v) in need.items():
                            if waited.get(key, 0) >= v:
                                continue
                            eng.wait_ge(h, v)
                            nw += 1
                            waited[key] = v
                        try:
                            bi = ins.fn(eng)
                        except BaseException as ex:
                            print("EMIT FAIL eng", e, "line", ins.line, "dma", ins.is_dma, repr(ex)[:2000], flush=True)
                            raise
                        if ins.is_dma:
                            bi.then_inc(ins.sem.h, 16)
                        elif ins.flag:
                            bi.then_inc(self.msem[e], 1)
                    if e == 'sp':
                        for (s, v) in final.values():
                            eng.wait_ge(s.h, v)
                    stats[e] = (len(lst), nw)
                getattr(block, engmap[e])(body)
        return stats


def build(stages=('l0mix', 'l0ffn', 'l1mix', 'l1ffn', 'final'), dbg=(), ne_run=NE, nfc_run=NFC):
    nc = bass.Bass("TRN2", target_bir_lowering=False)
    P = Prog(nc)
    dram = {}

    def din(name, shape):
        t = nc.dram_tensor(name, list(shape), F32, kind="ExternalInput").ap()
        dram[name] = t
        return t

    def dout(name, shape):
        return nc.dram_tensor(name, list(shape), F32, kind="ExternalOutput").ap()

    S = set(stages)
    xin = din("xin", [TT, D])
    cvec = din("cvec", [2, D])
    w_mod = din("w_mod", [2, D, 6 * D])
    b_mod = din("b_mod", [2, 6 * D])
    g_mix = din("g_mix", [2, D])
    g_ffn = din("g_ffn", [2, D])
    if 'l0mix' in S:
        cache_ckv = din("cache_ckv", [512, 512])
        cache_kr = din("cache_kr", [512, 64])
        w_in_a = din("w_in_a", [D, 4160])
        g_q = din("g_q", [512])
        g_kv = din("g_kv", [512])
        w_uq = din("w_uq", [512, 1536])
        w_ukv = din("w_ukv", [512, 2048])
        conv_w = din("conv_w", [3, 1024])
        w_o_a = din("w_o_a", [D, D])
        ropec = din("ropec", [64, TS])
        ropes = din("ropes", [64, TS])
        o_ckv = dout("o_ckv", [2, TP, 512])
        o_kr = dout("o_kr", [2, TP, 64])
    if 'l0ffn' in S:
        w_fg = din("w_fg", [D, DFF])
        w_fu = din("w_fu", [D, DFF])
        w_fd = din("w_fd", [DFF, D])
    if 'l1mix' in S:
        st_C = din("st_C", [2, 4, 256, 512])
        st_n = din("st_n", [2, 4, 256])
        st_m = din("st_m", [1, 8])
        w_in_c = din("w_in_c", [D, 6160])
        b_gates = din("b_gates", [1, 16])
        g_h = din("g_h", [1, D])
        w_o_c = din("w_o_c", [D, D])
        o_C = dout("o_C", [2, 2, 4, 256, 512])
        o_n = dout("o_n", [2, 2, 4, 256])
        o_m = dout("o_m", [2, 8])
    if 'l1ffn' in S:
        w_router = din("w_router", [D, NE])
        w_eg = din("w_eg", [ne_run, D, DFF])
        w_eu = din("w_eu", [ne_run, D, DFF])
        w_ed = din("w_ed", [ne_run, DFF, D])
    if 'final' in S:
        g_final = din("g_final", [D])
        yout = dout("yout", [TT, D])
    dbg_out = {}
    for name in dbg:
        if name in ('dg', 'lgt'):
            dbg_out[name] = dout("dbg_" + name, [128, 96])
        else:
            dbg_out[name] = dout("dbg_" + name, [128, NCH * TT])

    ARENA = 212800
    arena = nc.alloc_sbuf_tensor("arena", [128, ARENA // 4], F32).ap()

    def fview(off, n):
        assert off % 4 == 0 and off + 4 * n <= ARENA, (off, n)
        return arena[:, off // 4: off // 4 + n]

    def bview(off, n):
        assert off % 4 == 0 and n % 2 == 0 and off + 2 * n <= ARENA, (off, n)
        return arena[:, off // 4: off // 4 + n // 2].bitcast(BF16)

    O_X = 0
    MAXSLOT = 9
    nslot = [6]
    O_CONST = 98304
    O_MODV = O_CONST + 4096
    O_RING = O_MODV + 3072
    O_H = O_RING + 6 * 4096
    O_HF = O_RING + MAXSLOT * 4096
    SCR_END = ARENA

    xT = fview(O_X, NCH * TT).rearrange("p (c t) -> p c t", c=NCH)
    xT_b = [[Buf(f"x{c}_{g}") for g in range(3)] for c in range(NCH)]
    ring = [bview(O_RING + i * 4096, 2048) for i in range(MAXSLOT)]
    ring_b = [Buf(f"ring{i}") for i in range(MAXSLOT)]
    ring_s = [P.dsem(f"rs{i}") for i in range(MAXSLOT)]
    ring_i = [0]
    f32slab_s = [P.dsem(f"f32s{i}") for i in range(6)]

    ident_f = fview(O_CONST, 128)
    ident_b = bview(O_CONST + 512, 128)
    ones_b = bview(O_CONST + 768, 128)
    ones_f = fview(O_CONST + 1024, 128)
    triU = fview(O_CONST + 1536, 128)
    triL = fview(O_CONST + 2048, 128)
    negU = fview(O_CONST + 2560, 128)
    negL = fview(O_CONST + 3072, 128)
    cbuf = Buf("const")
    ps = [nc.alloc_psum_tensor(f"ps{i}", [128, 512], F32).ap() for i in range(8)]
    psb = [Buf(f"ps{i}") for i in range(8)]

    def PE(out, lhsT, rhs, start, stop, rd, wr, **kw):
        return P.op('pe', lambda e: e.matmul(out, lhsT, rhs, start=start, stop=stop, **kw), rd, wr)

    def TR(out, in_, idn, rd, wr):
        return P.op('pe', lambda e: e.transpose(out, in_, idn), rd, wr)

    def ACT(out, in_, func, rd, wr, **kw):
        return P.op('act', lambda e: e.activation(out=out, in_=in_, func=func, **kw), rd, wr)

    def V(eng, method, rd, wr, *a, **kw):
        return P.op(eng, lambda e: getattr(e, method)(*a, **kw), rd, wr)

    def DVE(method, rd, wr, *a, **kw):
        return V('dve', method, rd, wr, *a, **kw)

    def POOL(method, rd, wr, *a, **kw):
        return V('pool', method, rd, wr, *a, **kw)

    def slab(dst_fn, src):
        i = ring_i[0] % nslot[0]
        ring_i[0] += 1
        v = dst_fn(ring[i])
        P.dma('pool', v, src, ring_s[i], writes=[ring_b[i]])
        return v, ring_b[i]

    def kslab(w, c0, n):
        return slab(lambda r: r[:, 0:16 * n].rearrange("p (k c) -> p k c", k=16),
                    w[:, c0:c0 + n].rearrange("(k p) c -> p k c", p=128))

    POOL('memset', [], [cbuf], ident_f, 0.0)
    POOL('affine_select', [], [cbuf], out=ident_f, in_=ident_f, pattern=[[-1, 128]], compare_op=ALU.not_equal,
         fill=1.0, base=0, channel_multiplier=1)
    POOL('tensor_copy', [], [cbuf], out=ident_b, in_=ident_f)
    POOL('memset', [], [cbuf], ones_b, 1.0)
    POOL('memset', [], [cbuf], ones_f, 1.0)
    POOL('memset', [], [cbuf], triU, 1.0)
    POOL('affine_select', [], [cbuf], out=triU, in_=triU, pattern=[[1, 128]], compare_op=ALU.is_ge,
         fill=0.0, base=0, channel_multiplier=-1)
    POOL('memset', [], [cbuf], triL, 1.0)
    POOL('affine_select', [], [cbuf], out=triL, in_=triL, pattern=[[-1, 128]], compare_op=ALU.is_ge,
         fill=0.0, base=0, channel_multiplier=1)

    POOL('memset', [], [cbuf], negU, 0.0)
    POOL('affine_select', [], [cbuf], out=negU, in_=negU, pattern=[[1, 128]], compare_op=ALU.is_ge,
         fill=-30000.0, base=0, channel_multiplier=-1)
    POOL('memset', [], [cbuf], negL, 0.0)
    POOL('affine_select', [], [cbuf], out=negL, in_=negL, pattern=[[-1, 128]], compare_op=ALU.is_ge,
         fill=-30000.0, base=0, channel_multiplier=1)

    misc_s = [P.dsem(f"misc{i}", serial=True) for i in range(4)]
    misc_i = [0]

    def msem():
        misc_i[0] += 1
        return misc_s[misc_i[0] % 4]

    pmisc_s = [P.dsem(f"pmisc{i}", serial=True) for i in range(2)]
    pmisc_i = [0]

    def pmsem():
        pmisc_i[0] += 1
        return pmisc_s[pmisc_i[0] % 2]

    class Scr:
        def __init__(self, lo, hi):
            self.lo, self.hi, self.p = lo, hi, lo

        def ft(self, n):
            self.hi -= 4 * n
            assert self.p <= self.hi, ("scratch overflow", self.p, self.hi)
            return fview(self.hi, n)

        def bt(self, n):
            n2 = (n + 1) // 2 * 2
            self.hi -= 2 * n2
            assert self.p <= self.hi, ("scratch overflow", self.p, self.hi)
            return bview(self.hi, n2)[:, 0:n]

        def f(self, n):
            o = self.p
            self.p += 4 * n
            assert self.p <= self.hi, ("scratch overflow", self.p, self.hi)
            return fview(o, n)

        def b(self, n):
            n2 = (n + 1) // 2 * 2
            o = self.p
            self.p += 2 * n2
            assert self.p <= self.hi, ("scratch overflow", self.p, self.hi)
            return bview(o, n2)[:, 0:n]

    phase_buf = Buf("phase")

    def load_cols(dst, vec_rows, n, tmp):
        tmp_rows, tb = tmp
        P.dma('sp', tmp_rows[0:n, :], vec_rows, msem(), writes=[tb])
        TR(ps[7][:, 0:n], tmp_rows[0:n, :], ident_f[0:n, 0:n], [tb, cbuf], [psb[7]])
        b = Buf()
        DVE('tensor_copy', [], [psb[7], b], out=dst, in_=ps[7][:, 0:n])
        return b

    def load_x():
        P.barrier()
        sc = Scr(O_H, ARENA)
        stg = [sc.f(2048) for _ in range(2)]
        stg_b = [Buf() for _ in range(2)]
        stg_s = [P.dsem() for _ in range(2)]
        for t in range(TT // 128):
            s = t % 2
            g = t // 4
            P.dma('sp', stg[s], xin[t * 128:(t + 1) * 128, :], stg_s[s], writes=[stg_b[s]])
            for c4 in range(4):
                bk = (t * 4 + c4) % 8
                for j in range(4):
                    c = c4 * 4 + j
                    TR(ps[bk][:, j * 128:(j + 1) * 128], stg[s][:, c * 128:(c + 1) * 128], ident_f, [stg_b[s], cbuf], [psb[bk]])
                wr = [psb[bk]] + [xT_b[c4 * 4 + j][g] for j in range(4)]
                o = xT[:, c4 * 4:c4 * 4 + 4, t * 128:(t + 1) * 128]
                i = ps[bk].rearrange("p (j t) -> p j t", j=4)
                if c4 % 2 == 0:
                    DVE('tensor_copy', [], wr, out=o, in_=i)
                else:
                    P.op('act', lambda e, o=o, i=i: e.copy(out=o, in_=i), [], wr)

    modT = fview(O_MODV, 192).rearrange("p (f b) -> p f b", b=2)
    AB = fview(O_MODV + 768, 6 * 32).rearrange("p (m c b) -> p m c b", m=6, c=16)
    gv = fview(O_MODV + 768 + 768, 3 * 16).rearrange("p (m c) -> p m c", m=3)
    condT = fview(O_MODV + 768 + 768 + 192, 32).rearrange("p (b k) -> p b k", b=2)
    mod_b = Buf("mod")

    def adaln(l, first):
        P.barrier()
        sc = Scr(O_H, ARENA)
        tmpr = (sc.f(128), Buf())
        ctmp = sc.f(32)
        bm = sc.f(96)
        if first:
            cb = load_cols(ctmp, cvec.rearrange("b (k p) -> (b k) p", p=128), 32, tmpr)
            ACT(condT, ctmp.rearrange("p (b k) -> p b k", b=2), AF.Silu, [cb], [mod_b])
        bmb = load_cols(bm, b_mod[l].rearrange("(f p) -> f p", p=128), 96, tmpr)
        g1b = load_cols(gv[:, 0, :], g_mix[l].rearrange("(f p) -> f p", p=128), 16, tmpr)
        g2b = load_cols(gv[:, 1, :], g_ffn[l].rearrange("(f p) -> f p", p=128), 16, tmpr)
        rowb = [sc.f(512) for _ in range(2)]
        rowb_b = [Buf() for _ in range(2)]
        si = 0
        for cg in range(24):
            bk = cg % 2
            for k2 in range(8):
                j = si % 6
                si += 1
                wv = fview(O_RING + j * 4096, 1024).rearrange("p (kk c) -> p kk c", kk=2)
                P.dma('sp', wv, w_mod[l][k2 * 256:(k2 + 1) * 256, cg * 512:(cg + 1) * 512].rearrange("(kk p) c -> p kk c", p=128),
                      f32slab_s[j], writes=[ring_b[j]])
                for kk in range(2):
                    k = k2 * 2 + kk
                    PE(ps[bk][0:2, :], condT[:, :, k], wv[:, kk, :], k == 0, k == 15, [ring_b[j], mod_b], [psb[bk]])
            DVE('tensor_copy', [], [psb[bk], rowb_b[bk]], out=rowb[bk][0:2, :], in_=ps[bk][0:2, :])
            for jj in range(4):
                f = cg * 4 + jj
                TR(ps[6][:, 2 * f:2 * f + 2], rowb[bk][0:2, jj * 128:(jj + 1) * 128], ident_f[0:2, 0:2], [rowb_b[bk], cbuf], [psb[6]])
        DVE('tensor_tensor', [bmb], [psb[6], mod_b], out=modT, in0=ps[6][:, 0:192].rearrange("p (f b) -> p f b", b=2),
            in1=bm.unsqueeze(2).to_broadcast([128, 96, 2]), op=ALU.add)
        for b in range(2):
            DVE('scalar_tensor_tensor', [g1b], [mod_b], out=AB[:, 0, :, b], in0=modT[:, 16:32, b], scalar=1.0, in1=gv[:, 0, :],
                op0=ALU.add, op1=ALU.mult)
            DVE('tensor_copy', [], [mod_b], out=AB[:, 1, :, b], in_=modT[:, 0:16, b])
            DVE('tensor_copy', [], [mod_b], out=AB[:, 2, :, b], in_=modT[:, 32:48, b])
            DVE('scalar_tensor_tensor', [g2b], [mod_b], out=AB[:, 3, :, b], in0=modT[:, 64:80, b], scalar=1.0, in1=gv[:, 1, :],
                op0=ALU.add, op1=ALU.mult)
            DVE('tensor_copy', [], [mod_b], out=AB[:, 4, :, b], in_=modT[:, 48:64, b])
            DVE('tensor_copy', [], [mod_b], out=AB[:, 5, :, b], in_=modT[:, 80:96, b])

    def grp_of(tok):
        return tok // 512

    def modulate(hT, hT_b, tok0, T, ia, ib, bsel, sc, side=None):
        P.barrier()
        NG = max(1, T // 512)
        N = min(512, T)
        sq = [sc.b(512) for _ in range(2)]
        sq_b = [Buf() for _ in range(2)]
        rstd = sc.f(512)
        rstd_b = Buf()
        tmp = [sc.f(512) for _ in range(2)]
        tmp_b = [Buf() for _ in range(2)]
        if side is not None:
            sqf = [sc.f(512) for _ in range(2)]
        if side is not None and len(side) > 4:
            hs2 = [sc.b(512) for _ in range(2)]
            hs3 = [sc.b(512) for _ in range(2)]
            hs2_b = [Buf() for _ in range(2)]
            hs3_b = [Buf() for _ in range(2)]
        for gi in range(NG):
            t0 = tok0 + gi * N
            g = grp_of(t0)
            for c in range(NCH):
                q = c % 2
                if side is not None:
                    ACT(sqf[q][:, 0:N], xT[:, c, t0:t0 + N], AF.Square, [xT_b[c][g]], [sq_b[q]])
                    PE(ps[7][:, 0:N], ones_f, sqf[q][:, 0:N], c == 0, c == NCH - 1, [sq_b[q], cbuf], [psb[7]])
                else:
                    ACT(sq[q][:, 0:N], xT[:, c, t0:t0 + N], AF.Square, [xT_b[c][g]], [sq_b[q]])
                    PE(ps[7][:, 0:N], ones_b, sq[q][:, 0:N], c == 0, c == NCH - 1, [sq_b[q], cbuf], [psb[7]])
            if side is not None:
                vms = tmp[0]
                DVE('tensor_scalar', [], [psb[7], tmp_b[0]], out=vms[:, 0:N], in0=ps[7][:, 0:N], scalar1=1.0 / D, scalar2=EPS,
                    op0=ALU.mult, op1=ALU.add)
                P.op('act', lambda e, o=rstd[:, 0:N], i=vms[:, 0:N]: e.sqrt(out=o, in_=i), [tmp_b[0]], [rstd_b])
                DVE('reciprocal', [], [rstd_b], out=rstd[:, 0:N], in_=rstd[:, 0:N])
                DVE('tensor_tensor', [rstd_b], [tmp_b[0]], out=vms[:, 0:N], in0=vms[:, 0:N], in1=rstd[:, 0:N], op=ALU.mult)
                DVE('tensor_tensor', [rstd_b], [tmp_b[0]], out=vms[:, 0:N], in0=vms[:, 0:N], in1=rstd[:, 0:N], op=ALU.mult)
                DVE('tensor_scalar', [], [tmp_b[0]], out=vms[:, 0:N], in0=vms[:, 0:N], scalar1=-0.5, scalar2=1.5, op0=ALU.mult, op1=ALU.add)
                DVE('tensor_tensor', [tmp_b[0]], [rstd_b], out=rstd[:, 0:N], in0=rstd[:, 0:N], in1=vms[:, 0:N], op=ALU.mult)
            else:
                DVE('tensor_scalar', [], [psb[7], rstd_b], out=rstd[:, 0:N], in0=ps[7][:, 0:N], scalar1=1.0 / D, scalar2=EPS,
                    op0=ALU.mult, op1=ALU.add)
                P.op('act', lambda e, o=rstd[:, 0:N], i=rstd[:, 0:N]: e.sqrt(out=o, in_=i), [], [rstd_b])
                DVE('reciprocal', [], [rstd_b], out=rstd[:, 0:N], in_=rstd[:, 0:N])
            for c in range(NCH):
                q = c % 2
                DVE('scalar_tensor_tensor', [xT_b[c][g], rstd_b, mod_b], [tmp_b[q]], out=tmp[q][:, 0:N], in0=xT[:, c, t0:t0 + N],
                    scalar=AB[:, ia, c, bsel:bsel + 1], in1=rstd[:, 0:N], op0=ALU.mult, op1=ALU.mult)
                if side is not None:
                    w32, n, bank, wbuf = side[:4]
                    DVE('tensor_scalar', [mod_b], [tmp_b[q]], out=tmp[q][:, 0:N], in0=tmp[q][:, 0:N],
                        scalar1=AB[:, ib, c, bsel:bsel + 1], scalar2=None, op0=ALU.add)
                    P.op('act', lambda e, o=hT[:, c, gi * N:gi * N + N], i=tmp[q][:, 0:N]: e.copy(out=o, in_=i), [tmp_b[q]], [hT_b[c][gi]])
                    if len(side) > 4:
                        w1, w2, w3 = side[4]
                        h1 = hT[:, c, gi * N:gi * N + N]
                        DVE('tensor_tensor', [hT_b[c][gi]], [tmp_b[q]], out=tmp[q][:, 0:N], in0=tmp[q][:, 0:N], in1=h1, op=ALU.subtract)
                        P.op('act', lambda e, o=hs2[q][:, 0:N], i=tmp[q][:, 0:N]: e.copy(out=o, in_=i), [tmp_b[q]], [hs2_b[q]])
                        DVE('tensor_tensor', [hs2_b[q]], [tmp_b[q]], out=tmp[q][:, 0:N], in0=tmp[q][:, 0:N], in1=hs2[q][:, 0:N], op=ALU.subtract)
                        P.op('act', lambda e, o=hs3[q][:, 0:N], i=tmp[q][:, 0:N]: e.copy(out=o, in_=i), [tmp_b[q]], [hs3_b[q]])
                        terms = [(h1, [hT_b[c][gi]], 0, w1), (h1, [hT_b[c][gi]], 0, w2), (hs2[q], [hs2_b[q]], 1, w1), (hs2[q], [hs2_b[q]], 1, w2),
                                 (h1, [hT_b[c][gi]], 0, w3), (hs3[q], [hs3_b[q]], 1, w1)]
                        for tl in range(N // 128):
                            tile = gi * (N // 128) + tl
                            for ti, (ha, hab, loc, wa) in enumerate(terms):
                                PE(ps[bank][:, tile * n:(tile + 1) * n], ha[:, tl * 128:(tl + 1) * 128], wa[:, c, :],
                                   (c == 0 and tl == 0 and gi == 0 and ti == 0), (c == NCH - 1 and ti == len(terms) - 1), hab + [wbuf], [psb[bank]],
                                   skip_group_check=True)
                    else:
                        for tl in range(N // 128):
                            tile = gi * (N // 128) + tl
                            PE(ps[bank][:, tile * n:(tile + 1) * n], tmp[q][:, tl * 128:(tl + 1) * 128], w32[:, c, :],
                               (c == 0 and tl == 0 and gi == 0), (c == NCH - 1), [tmp_b[q], wbuf], [psb[bank]], skip_group_check=True)
                else:
                    ACT(hT[:, c, gi * N:gi * N + N], tmp[q][:, 0:N], AF.Identity, [tmp_b[q], mod_b], [hT_b[c][gi]],
                        bias=AB[:, ib, c, bsel:bsel + 1], scale=1.0)

    def resid_add(bank, n0, N, c, tok, ig, bsel):
        g = grp_of(tok)
        DVE('scalar_tensor_tensor', [mod_b], [psb[bank], xT_b[c][g]], out=xT[:, c, tok:tok + N], in0=ps[bank][:, n0:n0 + N],
            scalar=AB[:, ig, c, bsel:bsel + 1], in1=xT[:, c, tok:tok + N], op0=ALU.mult, op1=ALU.add)

    def out_proj(w_o, krow0, nk, rhsT, rhs_bufs, tok0, T, ig, bsel, banks=(4, 5)):
        N = min(512, T)
        slabs = [slab(lambda r: r, w_o[(krow0 + k) * 128:(krow0 + k + 1) * 128, :]) for k in range(nk)]
        i = 0
        for m in range(NCH):
            for gi in range(T // N):
                bk = banks[i % len(banks)]
                i += 1
                for k in range(nk):
                    wv, wb = slabs[k]
                    PE(ps[bk][:, 0:N], wv[:, m * 128:(m + 1) * 128], rhsT[:, k, gi * N:gi * N + N], k == 0, k == nk - 1,
                       [wb] + rhs_bufs, [psb[bk]])
                resid_add(bk, 0, N, m, tok0 + gi * N, ig, bsel)

    def l0_mixer(tok0, T, sample, seq):
        P.barrier()
        bsel = 1 if sample else 0
        N = min(512, T)
        NG = T // N
        Tk = T + (512 if sample else 0)
        R = Scr(O_H, ARENA)
        hT = R.b(NCH * T).rearrange("p (c t) -> p c t", c=NCH)
        hT_b = [[Buf() for _ in range(NG)] for _ in range(NCH)]
        cqn = R.bt(4 * T).rearrange("p (c t) -> p c t", c=4)
        ckvn = R.bt(4 * Tk).rearrange("p (c t) -> p c t", c=4)
        krT = R.bt(Tk)
        cq_b, ckv_b, kr_b = Buf(), Buf(), Buf()
        gq = R.ft(4)
        gkv = R.ft(4)
        cw = R.ft(24).rearrange("p (j c) -> p j c", j=3)
        tmpr = (R.ft(128), Buf())
        gq_b = load_cols(gq, g_q.rearrange("(f p) -> f p", p=128), 4, tmpr)
        gkv_b = load_cols(gkv, g_kv.rearrange("(f p) -> f p", p=128), 4, tmpr)
        cw_b = [load_cols(cw[:, j, :], conv_w[j].rearrange("(f p) -> f p", p=128), 8, tmpr) for j in range(3)]
        if sample:
            cosT = R.bt(TS)
            sinT = R.bt(TS)
            rope_b = Buf()
            P.dma('pool', cosT[0:64, :], ropec, pmsem(), writes=[rope_b])
            P.dma('pool', sinT[0:64, :], ropes, pmsem(), writes=[rope_b])
        mark = R.p
        SCR_HI = R.hi
        scm = Scr(mark, SCR_HI)
        modulate(hT, hT_b, tok0, T, 0, 1, bsel, scm)

        P.barrier()
        sc3 = Scr(mark, SCR_HI)
        sq = [sc3.b(512) for _ in range(2)]
        sq_b = [Buf() for _ in range(2)]
        rstd = sc3.f(512)
        rstd_b = Buf()
        f32o = sc3.f(512)
        f32o_b = Buf()
        ostg = [sc3.f(512) for _ in range(2)]
        ostg_b = [Buf() for _ in range(2)]
        ostg_s = [P.dsem() for _ in range(2)]
        oi = [0]
        for (col0, gvec, gb, dst, dst_b, is_kv) in ((0, gq, gq_b, cqn, cq_b, False), (C1, gkv, gkv_b, ckvn, ckv_b, True)):
            for gi in range(NG):
                for c in range(4):
                    wv, wb = kslab(w_in_a, col0 + c * 128, 128)
                    for k in range(NCH):
                        PE(ps[c][:, 0:N], wv[:, k, :], hT[:, k, gi * N:gi * N + N], k == 0, k == NCH - 1, [wb, hT_b[k][gi]], [psb[c]])
                    q = c % 2
                    ACT(sq[q][:, 0:N], ps[c][:, 0:N], AF.Square, [], [psb[c], sq_b[q]])
                    PE(ps[7][:, 0:N], ones_b, sq[q][:, 0:N], c == 0, c == 3, [sq_b[q], cbuf], [psb[7]])
                DVE('tensor_scalar', [], [psb[7], rstd_b], out=rstd[:, 0:N], in0=ps[7][:, 0:N], scalar1=1.0 / 512, scalar2=EPS,
                    op0=ALU.mult, op1=ALU.add)
                P.op('act', lambda e, o=rstd[:, 0:N], i=rstd[:, 0:N]: e.sqrt(out=o, in_=i), [], [rstd_b])
                DVE('reciprocal', [], [rstd_b], out=rstd[:, 0:N], in_=rstd[:, 0:N])
                for c in range(4):
                    if is_kv and not sample:
                        DVE('scalar_tensor_tensor', [gb, rstd_b], [psb[c], f32o_b], out=f32o[:, 0:N], in0=ps[c][:, 0:N],
                            scalar=gvec[:, c:c + 1], in1=rstd[:, 0:N], op0=ALU.mult, op1=ALU.mult)
                        P.op('act', lambda e, o=dst[:, c, gi * N:gi * N + N], i=f32o[:, 0:N]: e.copy(out=o, in_=i), [f32o_b], [dst_b])
                        for tl in range(N // 128):
                            TR(ps[4 + tl][:, c * 128:(c + 1) * 128], f32o[:, tl * 128:(tl + 1) * 128], ident_f, [f32o_b, cbuf], [psb[4 + tl]])
                    else:
                        DVE('scalar_tensor_tensor', [gb, rstd_b], [psb[c], dst_b], out=dst[:, c, gi * N:gi * N + N], in0=ps[c][:, 0:N],
                            scalar=gvec[:, c:c + 1], in1=rstd[:, 0:N], op0=ALU.mult, op1=ALU.mult)
                if is_kv and not sample:
                    for tl in range(N // 128):
                        s = oi[0] % 2
                        oi[0] += 1
                        DVE('tensor_copy', [], [psb[4 + tl], ostg_b[s]], out=ostg[s], in_=ps[4 + tl])
                        P.dma('sp', o_ckv[seq, gi * N + tl * 128: gi * N + (tl + 1) * 128, :], ostg[s], ostg_s[s], reads=[ostg_b[s]], is_output=True)
        krf = sc3.f(512)
        krf_b = Buf()
        krf2 = sc3.f(512)
        krf2_b = Buf()
        for gi in range(NG):
            wv, wb = kslab(w_in_a, C2, 64)
            for k in range(NCH):
                PE(ps[0][0:64, 0:N], wv[:, k, :], hT[:, k, gi * N:gi * N + N], k == 0, k == NCH - 1, [wb, hT_b[k][gi]], [psb[0]])
            if sample:
                i = ring_i[0] % nslot[0]
                ring_i[0] += 1
                wp = ring[i][:, 0:1024].rearrange("p (k c) -> p k c", k=16)
                wsrc = w_in_a[:, C2:C2 + 64].rearrange("(k p) c -> p k c", p=128)
                for a in range(2):
                    for b in range(2):
                        P.dma('pool', wp[:, :, a * 32 + b * 16: a * 32 + b * 16 + 16], wsrc[:, :, a * 32 + (1 - b) * 16: a * 32 + (1 - b) * 16 + 16],
                              ring_s[i], writes=[ring_b[i]])
                wpb = ring_b[i]
                for k in range(NCH):
                    PE(ps[1][0:64, 0:N], wp[:, k, :], hT[:, k, gi * N:gi * N + N], k == 0, k == NCH - 1, [wpb, hT_b[k][gi]], [psb[1]])
                DVE('tensor_tensor', [rope_b], [psb[0], krf_b], out=krf[0:64, 0:N], in0=ps[0][0:64, 0:N], in1=cosT[0:64, gi * N:gi * N + N], op=ALU.mult)
                DVE('tensor_tensor', [rope_b], [psb[1], krf2_b], out=krf2[0:64, 0:N], in0=ps[1][0:64, 0:N], in1=sinT[0:64, gi * N:gi * N + N], op=ALU.mult)
                POOL('tensor_tensor', [krf_b, krf2_b], [kr_b], out=krT[0:64, gi * N:gi * N + N], in0=krf[0:64, 0:N], in1=krf2[0:64, 0:N], op=ALU.add)
            else:
                DVE('tensor_copy', [], [psb[0], krf_b], out=krf[0:64, 0:N], in_=ps[0][0:64, 0:N])
                P.op('act', lambda e, o=krT[0:64, gi * N:gi * N + N], i=krf[0:64, 0:N]: e.copy(out=o, in_=i), [krf_b], [kr_b])
                for tl in range(N // 128):
                    TR(ps[2][:, tl * 64:(tl + 1) * 64], krf[0:64, tl * 128:(tl + 1) * 128], ident_f[0:64, 0:64], [krf_b, cbuf], [psb[2]])
                ob = Buf()
                DVE('tensor_copy', [], [psb[2], ob], out=krf2[:, 0:(N // 128) * 64], in_=ps[2][:, 0:(N // 128) * 64])
                for tl in range(N // 128):
                    P.dma('sp', o_kr[seq, gi * N + tl * 128: gi * N + (tl + 1) * 128, :], krf2[:, tl * 64:(tl + 1) * 64], msem(), reads=[ob], is_output=True)
        if sample:
            cst = [sc3.f(512) for _ in range(2)]
            cst_b = [Buf() for _ in range(2)]
            cst_s = [P.dsem() for _ in range(2)]
            for tl in range(4):
                s = tl % 2
                P.dma('sp', cst[s], cache_ckv[tl * 128:(tl + 1) * 128, :], cst_s[s], writes=[cst_b[s]])
                for c in range(4):
                    TR(ps[2][:, c * 128:(c + 1) * 128], cst[s][:, c * 128:(c + 1) * 128], ident_f, [cst_b[s], cbuf], [psb[2]])
                DVE('tensor_copy', [], [psb[2], ckv_b], out=ckvn[:, :, T + tl * 128:T + (tl + 1) * 128],
                    in_=ps[2].rearrange("p (c t) -> p c t", c=4))
            kb = Buf()
            P.dma('sp', krf[:, 0:256].rearrange("p (j d) -> p j d", j=4), cache_kr.rearrange("(j p) d -> p j d", p=128), msem(), writes=[krf_b, kb])
            for tl in range(4):
                TR(ps[3][0:64, tl * 128:(tl + 1) * 128], krf[:, tl * 64:(tl + 1) * 64], ident_f, [krf_b, cbuf], [psb[3]])
            DVE('tensor_copy', [], [psb[3], kr_b], out=krT[0:64, T:T + 512], in_=ps[3][0:64, :])

        P.barrier()
        sc4 = Scr(mark, SCR_HI)
        ppad = sc4.f(T + 2)
        ppad_b = Buf()
        ubs = sc4.f(T)
        ubs_b = Buf()
        uxs = sc4.f(512)
        uxs_b = Buf()
        oc = sc4.f(T)
        oc_b = Buf()
        mixc = sc4.b(T).rearrange("p (k t) -> p k t", k=1)
        mixc_b = Buf()
        POOL('memset', [], [ppad_b], ppad[:, 0:1], 0.0)
        POOL('memset', [], [ppad_b], ppad[:, T + 1:T + 2], 0.0)
        for c in range(8):
            for gi in range(NG):
                for j, (col0, bk) in enumerate(((C3, 0), (C4, 1), (C5, 2))):
                    wv, wb = kslab(w_in_a, col0 + c * 128, 128)
                    for k in range(NCH):
                        PE(ps[bk][:, 0:N], wv[:, k, :], hT[:, k, gi * N:gi * N + N], k == 0, k == NCH - 1, [wb, hT_b[k][gi]], [psb[bk]])
                P.op('act', lambda e, o=uxs[:, 0:N], i=ps[0][:, 0:N]: e.copy(out=o, in_=i), [], [psb[0], uxs_b])
                P.op('act', lambda e, o=ubs[:, gi * N:gi * N + N], i=ps[1][:, 0:N]: e.copy(out=o, in_=i), [], [psb[1], ubs_b])
                DVE('tensor_tensor', [uxs_b], [psb[2], ppad_b], out=ppad[:, 1 + gi * N:1 + gi * N + N], in0=ps[2][:, 0:N], in1=uxs[:, 0:N], op=ALU.mult)
            DVE('tensor_scalar', [ppad_b, cw_b[0]], [oc_b], out=oc, in0=ppad[:, 0:T], scalar1=cw[:, 0, c:c + 1], scalar2=None, op0=ALU.mult)
            DVE('scalar_tensor_tensor', [ppad_b, cw_b[1]], [oc_b], out=oc, in0=ppad[:, 1:T + 1], scalar=cw[:, 1, c:c + 1], in1=oc, op0=ALU.mult, op1=ALU.add)
            DVE('scalar_tensor_tensor', [ppad_b, cw_b[2]], [oc_b], out=oc, in0=ppad[:, 2:T + 2], scalar=cw[:, 2, c:c + 1], in1=oc, op0=ALU.mult, op1=ALU.add)
            POOL('tensor_tensor', [oc_b, ubs_b], [mixc_b], out=mixc[:, 0, :], in0=oc, in1=ubs, op=ALU.mult)
            out_proj(w_o_a, 8 + c, 1, mixc, [mixc_b], tok0, T, 2, bsel, banks=(4, 5))

        P.barrier()
        sc5 = Scr(O_H, SCR_HI)
        qn = sc5.b(T)
        qr = sc5.b(T)
        kn = sc5.b(Tk)
        NKT = Tk // 128
        vt = sc5.b(NKT * 128).rearrange("p (j e) -> p j e", e=128)
        pT = [sc5.b(512) for _ in range(2)]
        pT_b = [Buf() for _ in range(2)]
        rden = sc5.f(512)
        rden_b = Buf()
        atth = sc5.b(T).rearrange("p (k t) -> p k t", k=1)
        atth_b = Buf()
        qt1 = sc5.f(512)
        qt1_b = Buf()
        qt2 = sc5.f(512)
        qt2_b = Buf()
        qn_b, qr_b, kn_b, vt_b = Buf(), Buf(), Buf(), Buf()
        scale = 192.0 ** -0.5
        for h in range(8):
            i = ring_i[0] % nslot[0]
            ring_i[0] += 1
            wq = ring[i][:, 0:1024].rearrange("p (k c) -> p k c", k=4)
            wqs = w_uq[:, h * 192:(h + 1) * 192].rearrange("(k p) c -> p k c", p=128)
            P.dma('pool', wq[:, :, 0:192], wqs, ring_s[i], writes=[ring_b[i]])
            if sample:
                for a in range(2):
                    for b in range(2):
                        P.dma('pool', wq[:, :, 192 + a * 32 + b * 16:192 + a * 32 + b * 16 + 16],
                              wqs[:, :, 128 + a * 32 + (1 - b) * 16:128 + a * 32 + (1 - b) * 16 + 16], ring_s[i], writes=[ring_b[i]])
            wq_b = ring_b[i]
            wkv, wkv_b = slab(lambda r: r[:, 0:1024].rearrange("p (k c) -> p k c", k=4),
                              w_ukv[:, h * 256:(h + 1) * 256].rearrange("(k p) c -> p k c", p=128))
            for gi in range(NG):
                for k in range(4):
                    PE(ps[0][:, 0:N], wq[:, k, 0:128], cqn[:, k, gi * N:gi * N + N], k == 0, k == 3, [wq_b, cq_b], [psb[0]])
                P.op('act', lambda e, o=qn[:, gi * N:gi * N + N], i=ps[0][:, 0:N]: e.copy(out=o, in_=i), [], [psb[0], qn_b])
                for k in range(4):
                    PE(ps[1][0:64, 0:N], wq[:, k, 128:192], cqn[:, k, gi * N:gi * N + N], k == 0, k == 3, [wq_b, cq_b], [psb[1]])
                if sample:
                    for k in range(4):
                        PE(ps[2][0:64, 0:N], wq[:, k, 192:256], cqn[:, k, gi * N:gi * N + N], k == 0, k == 3, [wq_b, cq_b], [psb[2]])
                    DVE('tensor_tensor', [rope_b], [psb[1], qt1_b], out=qt1[0:64, 0:N], in0=ps[1][0:64, 0:N], in1=cosT[0:64, gi * N:gi * N + N], op=ALU.mult)
                    DVE('tensor_tensor', [rope_b], [psb[2], qt2_b], out=qt2[0:64, 0:N], in0=ps[2][0:64, 0:N], in1=sinT[0:64, gi * N:gi * N + N], op=ALU.mult)
                    POOL('tensor_tensor', [qt1_b, qt2_b], [qr_b], out=qr[0:64, gi * N:gi * N + N], in0=qt1[0:64, 0:N], in1=qt2[0:64, 0:N], op=ALU.add)
                else:
                    P.op('act', lambda e, o=qr[0:64, gi * N:gi * N + N], i=ps[1][0:64, 0:N]: e.copy(out=o, in_=i), [], [psb[1], qr_b])
            for kg in range((Tk + 511) // 512):
                n = min(512, Tk - kg * 512)
                for k in range(4):
                    PE(ps[3][:, 0:n], wkv[:, k, 0:128], ckvn[:, k, kg * 512:kg * 512 + n], k == 0, k == 3, [wkv_b, ckv_b], [psb[3]])
                P.op('act', lambda e, o=kn[:, kg * 512:kg * 512 + n], i=ps[3][:, 0:n]: e.copy(out=o, in_=i), [], [psb[3], kn_b])
            for j0 in range(0, NKT, 4):
                nj = min(4, NKT - j0)
                for jj in range(nj):
                    for k in range(4):
                        PE(ps[2][:, jj * 128:(jj + 1) * 128], ckvn[:, k, (j0 + jj) * 128:(j0 + jj + 1) * 128], wkv[:, k, 128:256], k == 0, k == 3,
                           [wkv_b, ckv_b], [psb[2]])
                DVE('tensor_copy', [], [psb[2], vt_b], out=vt[:, j0:j0 + nj, :], in_=ps[2][:, 0:nj * 128].rearrange("p (j e) -> p j e", e=128))
            for gi in range(NG):
                for kt in range(NKT):
                    sb = kt % 2
                    PE(ps[sb][:, 0:N], kn[:, kt * 128:(kt + 1) * 128], qn[:, gi * N:gi * N + N], True, False, [kn_b, qn_b], [psb[sb]])
                    PE(ps[sb][:, 0:N], krT[0:64, kt * 128:(kt + 1) * 128], qr[0:64, gi * N:gi * N + N], False, True, [kr_b, qr_b], [psb[sb]])
                    ACT(pT[sb][:, 0:N], ps[sb][:, 0:N], AF.Exp, [], [psb[sb], pT_b[sb]], scale=scale)
                    PE(ps[6][:, 0:N], ones_b, pT[sb][:, 0:N], kt == 0, kt == NKT - 1, [pT_b[sb], cbuf], [psb[6]])
                    PE(ps[7][:, 0:N], vt[:, kt, :], pT[sb][:, 0:N], kt == 0, kt == NKT - 1, [pT_b[sb], vt_b], [psb[7]])
                DVE('reciprocal', [], [psb[6], rden_b], out=rden[:, 0:N], in_=ps[6][:, 0:N])
                DVE('tensor_tensor', [rden_b], [psb[7], atth_b], out=atth[:, 0, gi * N:gi * N + N], in0=ps[7][:, 0:N], in1=rden[:, 0:N], op=ALU.mult)
            out_proj(w_o_a, h, 1, atth, [atth_b], tok0, T, 2, bsel, banks=(4, 5))

    def ffn_phase(layer):
        moe = (layer == 1)
        P.barrier()
        nslot[0] = MAXSLOT
        sc2 = Scr(O_HF, ARENA)
        hT = sc2.b(NCH * TT).rearrange("p (c t) -> p c t", c=NCH)
        hT_b = [[Buf() for _ in range(3)] for _ in range(NCH)]
        if moe:
            wr32 = sc2.f(NCH * NE).rearrange("p (k e) -> p k e", e=NE)
            wr_b = Buf()
            P.dma('sp', wr32, w_router.rearrange("(k p) e -> p k e", p=128), msem(), writes=[wr_b])
            dg = sc2.f(12 * NE).rearrange("p (t e) -> p t e", e=NE)
            dg_b = Buf()
            mx8 = sc2.f(8)
            mx_b = Buf()
            lg = sc2.f(NE)
            lg_b = Buf()
            den1 = sc2.f(1)
            rep = sc2.f(128)
            rep_b = Buf()
        mark = sc2.p
        for (tok0, T, bsel) in ((0, 512, 0), (512, 1024, 1)):
            scm = Scr(mark, SCR_END)
            sub_hT = hT[:, :, tok0:tok0 + T]
            sub_b = [[hT_b[c][grp_of(tok0) + gi] for gi in range(T // 512)] for c in range(NCH)]
            if moe:
                bank = 5 if tok0 == 0 else 4
                modulate(sub_hT, sub_b, tok0, T, 3, 4, bsel, scm, side=(wr32, NE, bank, wr_b))
                for tl in range(T // 128):
                    tile = tok0 // 128 + tl
                    lgv = ps[bank][:, tl * NE:(tl + 1) * NE]
                    DVE('tensor_copy', [], [psb[bank], lg_b], out=lg, in_=lgv)
                    if 'lgt' in dbg_out:
                        P.dma('sp', dbg_out['lgt'][:, tile * NE:(tile + 1) * NE], lg, msem(), reads=[lg_b], is_output=True)
                    DVE('max', [lg_b], [mx_b], out=mx8, in_=lg)
                    DVE('tensor_scalar', [mx_b], [lg_b, dg_b], out=dg[:, tile, :], in0=lg, scalar1=mx8[:, 1:2], scalar2=None, op0=ALU.is_ge)
                    DVE('tensor_scalar', [mx_b], [lg_b], out=lg, in0=lg, scalar1=mx8[:, 0:1], scalar2=None, op0=ALU.subtract)
                    ACT(lg, lg, AF.Exp, [], [lg_b])
                    DVE('tensor_tensor', [lg_b], [dg_b], out=dg[:, tile, :], in0=dg[:, tile, :], in1=lg, op=ALU.mult)
                    DVE('reduce_sum', [dg_b], [mx_b], out=den1, in_=dg[:, tile, :], axis=AX.X)
                    DVE('reciprocal', [], [mx_b], out=den1, in_=den1)
                    DVE('tensor_scalar', [mx_b], [dg_b], out=dg[:, tile, :], in0=dg[:, tile, :], scalar1=den1[:, 0:1], scalar2=None, op0=ALU.mult)
            else:
                modulate(sub_hT, sub_b, tok0, T, 3, 4, bsel, scm)
        if moe and 'dg' in dbg_out:
            P.dma('sp', dbg_out['dg'], dg.rearrange("p t e -> p (t e)"), msem(), reads=[dg_b], is_output=True)
        P.barrier()
        sc3 = Scr(mark, SCR_END)
        sg = [sc3.f(512) for _ in range(2)]
        sg_b = [Buf() for _ in range(2)]
        actT = [[sc3.b(TT) for _ in range(2)] for _ in range(2)]
        actT_b = [[Buf() for _ in range(2)] for _ in range(2)]
        if moe:
            dgT = sc3.b(TT)
            dgT_b = Buf()
        nexp = ne_run if moe else 1
        assert nfc_run % 2 == 0
        prev = [None]

        def down_groups(lo, hi):
            if prev[0] is None:
                return
            sds, a = prev[0]
            for idx in range(lo, hi):
                m, g = divmod(idx, 3)
                bk = 4 + idx % 4
                for j in range(2):
                    sdv, sdb = sds[j]
                    PE(ps[bk], sdv[:, m * 128:(m + 1) * 128], actT[a][j][:, g * 512:(g + 1) * 512], j == 0, j == 1, [sdb, actT_b[a][j]], [psb[bk]])
                resid_add(bk, 0, 512, m, g * 512, 5, 0 if g == 0 else 1)

        def bcast_gate(e_, tiles):
            for tile in tiles:
                DVE('tensor_scalar', [dg_b, cbuf], [rep_b], out=rep, in0=ones_f, scalar1=dg[:, tile, e_:e_ + 1], scalar2=None, op0=ALU.mult)
                bk = 4 + tile % 4
                PE(ps[bk][:, 0:128], rep, ident_f, True, True, [rep_b, cbuf], [psb[bk]])
                P.op('act', lambda en, o=dgT[:, tile * 128:(tile + 1) * 128], i=ps[bk][:, 0:128]: en.copy(out=o, in_=i), [], [psb[bk], dgT_b])

        pi = 0
        for e in range(nexp):
            if moe:
                if e == 0:
                    bcast_gate(0, range(12))
                wg_, wu_, wd_ = w_eg[e], w_eu[e], w_ed[e]
            else:
                wg_, wu_, wd_ = w_fg, w_fu, w_fd
            for p in range(nfc_run // 2):
                a = pi % 2
                pi += 1
                gus = []
                for j in range(2):
                    f = 2 * p + j
                    gus.append((kslab(wg_, f * 128, 128), kslab(wu_, f * 128, 128)))
                sds = [slab(lambda r: r, wd_[(2 * p + j) * 128:(2 * p + j + 1) * 128, :]) for j in range(2)]
                blk = 0
                for j in range(2):
                    (sgv, sgb), (suv, sub) = gus[j]
                    for g in range(3):
                        bg, bu = (0, 1) if blk % 2 == 0 else (2, 3)
                        for k in range(NCH):
                            PE(ps[bg], sgv[:, k, :], hT[:, k, g * 512:(g + 1) * 512], k == 0, k == NCH - 1, [sgb, hT_b[k][g]], [psb[bg]])
                            if k % 4 == 3:
                                down_groups(blk * 8 + k // 4, blk * 8 + k // 4 + 1)
                        for k in range(NCH):
                            PE(ps[bu], suv[:, k, :], hT[:, k, g * 512:(g + 1) * 512], k == 0, k == NCH - 1, [sub, hT_b[k][g]], [psb[bu]])
                            if k % 4 == 3:
                                down_groups(blk * 8 + 4 + k // 4, blk * 8 + 4 + k // 4 + 1)
                        q = blk % 2
                        ACT(sg[q], ps[bg], AF.Silu, [], [psb[bg], sg_b[q]])
                        if moe:
                            DVE('tensor_tensor', [dgT_b], [sg_b[q]], out=sg[q], in0=sg[q], in1=dgT[:, g * 512:(g + 1) * 512], op=ALU.mult)
                        DVE('tensor_tensor', [sg_b[q]], [psb[bu], actT_b[a][j]], out=actT[a][j][:, g * 512:(g + 1) * 512], in0=ps[bu], in1=sg[q], op=ALU.mult)
                        blk += 1
                        if moe and j == 1 and p == nfc_run // 2 - 1 and e + 1 < nexp:
                            bcast_gate(e + 1, range(4 * g, 4 * g + 4))
                prev[0] = (sds, a)
        down_groups(0, 48)
        nslot[0] = 6

    def final_phase():
        P.barrier()
        sc = Scr(O_H, ARENA)
        stg = [sc.f(2048) for _ in range(2)]
        stg_b = [Buf() for _ in range(2)]
        stg_s = [P.dsem() for _ in range(2)]
        sq = [sc.b(512) for _ in range(2)]
        sq_b = [Buf() for _ in range(2)]
        rstd = sc.f(512)
        rstd_b = Buf()
        tmp = [sc.f(512) for _ in range(2)]
        tmp_b = [Buf() for _ in range(2)]
        tmpr = (sc.f(128), Buf())
        gfb = load_cols(gv[:, 2, :], g_final.rearrange("(f p) -> f p", p=128), 16, tmpr)
        for g in range(3):
            for c in range(NCH):
                q = c % 2
                ACT(sq[q], xT[:, c, g * 512:(g + 1) * 512], AF.Square, [xT_b[c][g]], [sq_b[q]])
                PE(ps[7], ones_b, sq[q], c == 0, c == NCH - 1, [sq_b[q], cbuf], [psb[7]])
            DVE('tensor_scalar', [], [psb[7], rstd_b], out=rstd, in0=ps[7], scalar1=1.0 / D, scalar2=EPS, op0=ALU.mult, op1=ALU.add)
            P.op('act', lambda e, o=rstd, i=rstd: e.sqrt(out=o, in_=i), [], [rstd_b])
            DVE('reciprocal', [], [rstd_b], out=rstd, in_=rstd)
            for c in range(NCH):
                q = c % 2
                DVE('scalar_tensor_tensor', [xT_b[c][g], rstd_b, gfb], [tmp_b[q]], out=tmp[q], in0=xT[:, c, g * 512:(g + 1) * 512],
                    scalar=gv[:, 2, c:c + 1], in1=rstd, op0=ALU.mult, op1=ALU.mult)
                bk = c % 4
                for tl in range(4):
                    TR(ps[bk][:, tl * 128:(tl + 1) * 128], tmp[q][:, tl * 128:(tl + 1) * 128], ident_f, [tmp_b[q], cbuf], [psb[bk]])
                s = c % 2
                if c % 2 == 0:
                    DVE('tensor_copy', [], [psb[bk], stg_b[s]], out=stg[s][:, 0:512], in_=ps[bk])
                else:
                    P.op('act', lambda e, o=stg[s][:, 0:512], i=ps[bk]: e.copy(out=o, in_=i), [], [psb[bk], stg_b[s]])
                dst = yout[g * 512:(g + 1) * 512, c * 128:(c + 1) * 128].rearrange("(j p) f -> p j f", p=128)
                P.dma('sp', dst, stg[s][:, 0:512].rearrange("p (j f) -> p j f", j=4), stg_s[s], reads=[stg_b[s]], is_output=True)


    LN16 = math.log(16.0)

    def l1_mixer(tok0, T, sample, seq):
        P.barrier()
        bsel = 1 if sample else 0
        N = min(512, T)
        NG = T // N
        NT = T // 128
        QN = 256
        NQ = T // QN
        nslot[0] = 8
        bigsel = [0]
        R = Scr(O_RING + 8 * 4096, ARENA)
        hT = R.b(NCH * T).rearrange("p (c t) -> p c t", c=NCH)
        hT_b = [[Buf() for _ in range(NG)] for _ in range(NCH)]
        gt = R.ft(NT * 16).rearrange("p (t g) -> p t g", g=16)
        lf8 = R.ft(NT * 8).rearrange("p (t g) -> p t g", g=8)
        Btm = R.ft(NT * 8).rearrange("p (t g) -> p t g", g=8)
        u0 = R.ft(NT * 8).rearrange("p (t g) -> p t g", g=8)
        uu = R.ft(NT * 8).rearrange("p (t g) -> p t g", g=8)
        wint = R.ft(NT * 8).rearrange("p (t g) -> p t g", g=8)
        wgt = R.ft(NT * 8).rearrange("p (t g) -> p t g", g=8)
        wg32 = R.ft(16 * 16).rearrange("p (k g) -> p k g", g=16)
        bg = R.ft(16)
        m0b = R.ft(8)
        ghb = R.ft(512)
        sm = R.ft(16)
        g_b, wg_b, bg_b, m0_b, gh_b, sm_b = Buf(), Buf(), Buf(), Buf(), Buf(), Buf()
        P.dma('sp', wg32, w_in_c[:, 6144:6160].rearrange("(k p) g -> p k g", p=128), msem(), writes=[wg_b])
        P.dma('sp', bg, b_gates[0:1, :].to_broadcast([128, 16]), msem(), writes=[bg_b])
        if sample:
            P.dma('sp', m0b, st_m[0:1, :].to_broadcast([128, 8]), msem(), writes=[m0_b])
        mark = R.p
        HI = R.hi
        scm = Scr(mark, HI)
        modulate(hT, hT_b, tok0, T, 0, 1, bsel, scm, side=(wg32, 16, 5, wg_b))
        P.barrier()
        DVE('tensor_tensor', [bg_b], [psb[5], g_b], out=gt, in0=ps[5][:, 0:NT * 16].rearrange("p (t g) -> p t g", g=16),
            in1=bg.unsqueeze(1).to_broadcast([128, NT, 16]), op=ALU.add)
        for d, c0 in ((0, 4), (1, 12)):
            v = gt[:, :, c0:c0 + 4]
            o = lf8[:, :, d * 4:d * 4 + 4]
            ACT(o, v, AF.Exp, [g_b], [sm_b], scale=-1.0)
            DVE('tensor_scalar', [], [sm_b], out=o, in0=o, scalar1=1.0, scalar2=None, op0=ALU.add)
            ACT(o, o, AF.Ln, [], [sm_b])
            DVE('tensor_scalar', [], [sm_b], out=o, in0=o, scalar1=-1.0, scalar2=None, op0=ALU.mult)
        for j in range(NT):
            seqs = [(i, ones_f) for i in range(j)] + [(j, triU)]
            for n_, (i, L) in enumerate(seqs):
                PE(ps[6][:, j * 8:j * 8 + 4], L, lf8[:, i, 0:4], n_ == 0, n_ == len(seqs) - 1, [sm_b, cbuf], [psb[6]])
            seqs = [(j, triL)] + [(i, ones_f) for i in range(j + 1, NT)]
            for n_, (i, L) in enumerate(seqs):
                PE(ps[6][:, j * 8 + 4:j * 8 + 8], L, lf8[:, i, 4:8], n_ == 0, n_ == len(seqs) - 1, [sm_b, cbuf], [psb[6]])
        B_b = Buf()
        DVE('tensor_copy', [], [psb[6], B_b], out=Btm, in_=ps[6][:, 0:NT * 8].rearrange("p (t g) -> p t g", g=8))
        u_b = Buf()
        DVE('tensor_tensor', [g_b, B_b], [u_b], out=u0[:, :, 0:4], in0=gt[:, :, 0:4], in1=Btm[:, :, 0:4], op=ALU.subtract)
        DVE('tensor_tensor', [g_b, B_b], [u_b], out=u0[:, :, 4:8], in0=gt[:, :, 8:12], in1=Btm[:, :, 4:8], op=ALU.subtract)
        DVE('tensor_scalar', [], [u_b], out=uu, in0=u0, scalar1=-LN16, scalar2=None, op0=ALU.add)
        if sample:
            DVE('tensor_tensor', [B_b, m0_b], [u_b], out=wint, in0=Btm, in1=m0b.unsqueeze(1).to_broadcast([128, NT, 8]), op=ALU.add)
            ACT(wint, wint, AF.Exp, [], [u_b])
        else:
            for j in range(NT):
                TR(ps[7][0:8, j * 128:(j + 1) * 128], u0[:, j, :], ident_f, [u_b, cbuf], [psb[7]])
            mx = sm[0:8, 0:1]
            mm = sm[0:8, 1:2]
            mv = sm[0:8, 2:3]
            dg8 = sm[0:8, 8:16]
            DVE('reduce_max', [], [psb[7], sm_b], out=mx, in_=ps[7][0:8, 0:T], axis=AX.X)
            DVE('tensor_scalar', [], [sm_b], out=mm, in0=mx, scalar1=0.0, scalar2=None, op0=ALU.max)
            for j in range(NT):
                PE(ps[6][0:8, 64:65], lf8[:, j, :], ones_f[:, 0:1], j == 0, j == NT - 1, [sm_b, cbuf], [psb[6]])
            DVE('tensor_tensor', [], [psb[6], sm_b], out=mv, in0=ps[6][0:8, 64:65], in1=mm, op=ALU.add)
            P.dma('sp', o_m[seq:seq + 1, :].rearrange("a d -> d a"), mv, msem(), reads=[sm_b], is_output=True)
            DVE('tensor_scalar', [cbuf], [sm_b], out=dg8, in0=ident_f[0:8, 0:8], scalar1=mm, scalar2=None, op0=ALU.mult)
            PE(ps[6][:, 72:80], ones_f[0:8, :], dg8, True, True, [sm_b, cbuf], [psb[6]])
            DVE('tensor_tensor', [u_b], [psb[6], u_b], out=wgt, in0=uu, in1=ps[6][:, 72:80].unsqueeze(1).to_broadcast([128, NT, 8]), op=ALU.subtract)
            ACT(wgt, wgt, AF.Exp, [], [u_b])

        for h in range(4):
            P.barrier()
            sc = Scr(mark, HI)
            qT = sc.b(2 * T).rearrange("p (k t) -> p k t", k=2)
            kT = sc.b(2 * T).rearrange("p (k t) -> p k t", k=2)
            vtm = sc.b(NT * 512).rearrange("p (t e) -> p t e", e=512)
            q_b, k_b, v_b = Buf(), Buf(), Buf()
            if not sample:
                ktm = sc.b(NT * 256).rearrange("p (t e) -> p t e", e=256)
                kw = sc.b(256)
                ktm_b, kw_b = Buf(), Buf()
                cst = [sc.f(512) for _ in range(2)]
                cst_b = [Buf() for _ in range(2)]
                cst_s = [P.dsem() for _ in range(2)]
                nst = sc.f(4)
                nst_b = Buf()
            else:
                C0 = sc.b(1024).rearrange("p (k e) -> p k e", k=2)
                n0 = sc.b(2)
                C0_b = Buf()
            sig = sc.b(2 * 512).rearrange("p (t e) -> p t e", e=512)
            sig_b = Buf()
            Bbc = sc.f(QN)
            Bbc_b = Buf()
            rep = sc.f(128)
            rep_b = Buf()
            hacc = sc.f(2 * 512).rearrange("p (t e) -> p t e", e=512)
            hacc_b = Buf()
            W = sc.f(QN)
            W_b = Buf()
            ST = [sc.b(QN) for _ in range(2)]
            ST_b = [Buf() for _ in range(2)]
            dtmp = sc.f(128)
            dtmp_b = Buf()
            t1 = sc.f(512)
            t1_b = Buf()
            ybf = sc.b(512)
            ybf_b = Buf()
            yT = sc.b(4 * QN).rearrange("p (k t) -> p k t", k=4)
            yT_b = Buf()
            rr = sc.f(8)
            rr_b = Buf()
            P.dma('sp', ghb, g_h[0:1, h * 512:(h + 1) * 512].to_broadcast([128, 512]), msem(), writes=[gh_b])
            for (dst, dstb, cbase) in ((qT, q_b, h * 256), (kT, k_b, 1024 + h * 256)):
                for kc in range(2):
                    for gi in range(NG):
                        wv, wb = kslab(w_in_c, cbase + kc * 128, 128)
                        for k in range(NCH):
                            PE(ps[0][:, 0:N], wv[:, k, :], hT[:, k, gi * N:gi * N + N], k == 0, k == NCH - 1, [wb, hT_b[k][gi]], [psb[0]])
                        P.op('act', lambda e, o=dst[:, kc, gi * N:gi * N + N], i=ps[0][:, 0:N]: e.copy(out=o, in_=i), [], [psb[0], dstb])
            def big_slab(c0, ncols):
                ns = ncols // 128
                s0 = 4 * (bigsel[0] % 2)
                bigsel[0] += 1
                v = bview(O_RING + s0 * 4096, 2048 * ns).rearrange("p (k c) -> p k c", k=16)
                P.dma('pool', v, w_in_c[:, c0:c0 + ncols].rearrange("(k p) c -> p k c", p=128), ring_s[s0], writes=[ring_b[s0 + i_] for i_ in range(ns)])
                ring_i[0] = (s0 + 4) % 8
                return v, [ring_b[s0 + i_] for i_ in range(ns)]

            def tm_proj(c0, ncols, tiles, evac):
                wv, wbs = big_slab(c0, ncols)
                for jl, j in enumerate(tiles):
                    bk = 1 + jl % 2
                    for k in range(NCH):
                        PE(ps[bk][:, 0:ncols], hT[:, k, j * 128:(j + 1) * 128], wv[:, k, :], k == 0, k == NCH - 1,
                           wbs + [hT_b[k][(j * 128) // N]], [psb[bk]])
                    evac(jl, j, bk)
            tm_proj(2048 + h * 512, 512, range(NT), lambda jl, j, bk: P.op('act', lambda e, o=vtm[:, j, :], i=ps[bk]: e.copy(out=o, in_=i), [], [psb[bk], v_b]))
            if not sample:
                tm_proj(1024 + h * 256, 256, range(NT), lambda jl, j, bk: DVE('tensor_copy', [], [psb[bk], ktm_b], out=ktm[:, j, :], in_=ps[bk][:, 0:256]))
            for qg in range(NQ):
                jt0 = qg * 2
                tm_proj(4096 + h * 512, 512, [jt0, jt0 + 1],
                        lambda jl, j, bk: ACT(sig[:, jl, :], ps[bk], AF.Sigmoid, [], [psb[bk], sig_b]))
                for d in range(2):
                    col = d * 4 + h
                    for jl in range(2):
                        DVE('tensor_scalar', [B_b, cbuf], [rep_b], out=rep, in0=ones_f, scalar1=Btm[:, jt0 + jl, col:col + 1], scalar2=None, op0=ALU.mult)
                        PE(ps[7][:, jl * 128:(jl + 1) * 128], rep, ident_f, True, True, [rep_b, cbuf], [psb[7]])
                    DVE('tensor_copy', [], [psb[7], Bbc_b], out=Bbc, in_=ps[7][:, 0:QN])
                    if sample:
                        P.dma('pool', C0, st_C[d, h].rearrange("(k p) e -> p k e", p=128), pmsem(), writes=[C0_b])
                        P.dma('pool', n0.rearrange("p (k o) -> p k o", o=1), st_n[d, h].rearrange("(k p o) -> p k o", p=128, o=1), pmsem(), writes=[C0_b], allow_slow_non_contiguous=True)
                    sts = list(range(0, jt0 + 2)) if d == 0 else list(range(jt0, NT))
                    first_den = [True]
                    for si, st in enumerate(sts):
                        sb = 5 + si % 2
                        for kc in range(2):
                            PE(ps[sb][:, 0:QN], kT[:, kc, st * 128:(st + 1) * 128], qT[:, kc, qg * QN:(qg + 1) * QN], kc == 0, kc == 1, [k_b, q_b], [psb[sb]])
                        if d == 0:
                            val = [jl for jl in range(2) if jt0 + jl >= st]
                        else:
                            val = [jl for jl in range(2) if jt0 + jl <= st]
                        for jl in val:
                            cs = slice(jl * 128, (jl + 1) * 128)
                            if jt0 + jl == st:
                                DVE('tensor_tensor', [Bbc_b, cbuf], [dtmp_b], out=dtmp, in0=Bbc[:, cs], in1=(negU if d == 0 else negL), op=ALU.add)
                                ACT(W[:, cs], dtmp, AF.Exp, [dtmp_b, u_b], [W_b], bias=uu[:, st, col:col + 1], scale=1.0)
                            else:
                                ACT(W[:, cs], Bbc[:, cs], AF.Exp, [Bbc_b, u_b], [W_b], bias=uu[:, st, col:col + 1], scale=1.0)
                        c0, c1 = val[0] * 128, (val[-1] + 1) * 128
                        q = si % 2
                        DVE('tensor_tensor', [W_b], [psb[sb], ST_b[q]], out=ST[q][:, c0:c1], in0=ps[sb][:, c0:c1], in1=W[:, c0:c1], op=ALU.mult)
                        for jl in val:
                            jt = jt0 + jl
                            if d == 0:
                                fst, lst = (st == 0), (st == jt)
                            else:
                                fst, lst = (st == jt), (st == NT - 1)
                            PE(ps[jl], ST[q][:, jl * 128:(jl + 1) * 128], vtm[:, st, :], fst, lst, [ST_b[q], v_b], [psb[jl]])
                            PE(ps[4][:, jl:jl + 1], ST[q][:, jl * 128:(jl + 1) * 128], ones_b[:, 0:1], first_den[0], lst, [ST_b[q], cbuf], [psb[4]],
                               skip_group_check=True)
                            first_den[0] = False
                    for jl in range(2):
                        jt = jt0 + jl
                        den = rr[:, 0:1]
                        wr_ = rr[:, 1:2]
                        if sample:
                            for kc in range(2):
                                PE(ps[5], qT[:, kc, jt * 128:(jt + 1) * 128], C0[:, kc, :], kc == 0, kc == 1, [q_b, C0_b], [psb[5]])
                            for kc in range(2):
                                PE(ps[6][:, 0:1], qT[:, kc, jt * 128:(jt + 1) * 128], n0[:, kc:kc + 1], kc == 0, kc == 1, [q_b, C0_b], [psb[6]])
                            DVE('tensor_copy', [], [psb[6], rr_b], out=wr_, in_=ps[6][:, 0:1])
                            DVE('scalar_tensor_tensor', [u_b], [psb[4], rr_b], out=den, in0=wr_, scalar=wint[:, jt, col:col + 1], in1=ps[4][:, jl:jl + 1],
                                op0=ALU.mult, op1=ALU.add)
                        else:
                            DVE('tensor_copy', [], [psb[4], rr_b], out=den, in_=ps[4][:, jl:jl + 1])
                        DVE('tensor_scalar', [], [rr_b], out=rr[:, 3:4], in0=den, scalar1=-1.0, scalar2=None, op0=ALU.mult)
                        DVE('tensor_tensor', [], [rr_b], out=den, in0=den, in1=rr[:, 3:4], op=ALU.max)
                        DVE('tensor_scalar', [], [rr_b], out=den, in0=den, scalar1=1.0, scalar2=None, op0=ALU.max)
                        DVE('reciprocal', [], [rr_b], out=den, in_=den)
                        if d == 0:
                            DVE('tensor_scalar', [rr_b], [psb[jl], hacc_b], out=hacc[:, jl, :], in0=ps[jl], scalar1=den, scalar2=None, op0=ALU.mult)
                        else:
                            DVE('scalar_tensor_tensor', [rr_b], [psb[jl], hacc_b], out=hacc[:, jl, :], in0=ps[jl], scalar=den, in1=hacc[:, jl, :],
                                op0=ALU.mult, op1=ALU.add)
                        if sample:
                            DVE('tensor_tensor', [u_b], [rr_b], out=wr_, in0=den, in1=wint[:, jt, col:col + 1], op=ALU.mult)
                            DVE('scalar_tensor_tensor', [rr_b], [psb[5], hacc_b], out=hacc[:, jl, :], in0=ps[5], scalar=wr_, in1=hacc[:, jl, :],
                                op0=ALU.mult, op1=ALU.add)
                for jl in range(2):
                    ssq = rr[:, 2:3]
                    ACT(t1, hacc[:, jl, :], AF.Square, [hacc_b], [t1_b, rr_b], accum_out=ssq)
                    DVE('tensor_scalar', [], [rr_b], out=ssq, in0=ssq, scalar1=1.0 / 512, scalar2=EPS, op0=ALU.mult, op1=ALU.add)
                    P.op('act', lambda e, o=ssq, i=ssq: e.sqrt(out=o, in_=i), [], [rr_b])
                    DVE('reciprocal', [], [rr_b], out=ssq, in_=ssq)
                    DVE('scalar_tensor_tensor', [hacc_b, rr_b, gh_b], [t1_b], out=t1, in0=hacc[:, jl, :], scalar=ssq, in1=ghb, op0=ALU.mult, op1=ALU.mult)
                    DVE('tensor_tensor', [sig_b], [t1_b, ybf_b], out=ybf, in0=t1, in1=sig[:, jl, :], op=ALU.mult)
                    pb = ps[7].bitcast(BF16)
                    for ec in range(4):
                        TR(pb[:, ec * 128:(ec + 1) * 128], ybf[:, ec * 128:(ec + 1) * 128], ident_b, [ybf_b, cbuf], [psb[7]])
                    DVE('tensor_copy', [], [psb[7], yT_b], out=yT[:, :, jl * 128:(jl + 1) * 128], in_=pb[:, 0:512].rearrange("p (k t) -> p k t", k=4))
                out_proj(w_o_c, 4 * h, 4, yT, [yT_b], tok0 + qg * QN, QN, 2, bsel, banks=(5, 6))
            if not sample:
                oi = 0
                for d in range(2):
                    col = d * 4 + h
                    for dc in range(2):
                        for j in range(NT):
                            DVE('tensor_scalar', [ktm_b, u_b], [kw_b], out=kw[:, 0:128], in0=ktm[:, j, dc * 128:(dc + 1) * 128],
                                scalar1=wgt[:, j, col:col + 1], scalar2=None, op0=ALU.mult)
                            PE(ps[0], kw[:, 0:128], vtm[:, j, :], j == 0, j == NT - 1, [kw_b, v_b], [psb[0]])
                            PE(ps[1][:, 0:1], kw[:, 0:128], ones_b[:, 0:1], j == 0, j == NT - 1, [kw_b, cbuf], [psb[1]])
                        s = oi % 2
                        oi += 1
                        DVE('tensor_copy', [], [psb[0], cst_b[s]], out=cst[s], in_=ps[0])
                        P.dma('sp', o_C[seq, d, h, dc * 128:(dc + 1) * 128, :], cst[s], cst_s[s], reads=[cst_b[s]], is_output=True)
                        DVE('tensor_copy', [], [psb[1], nst_b], out=nst[:, 0:1], in_=ps[1][:, 0:1])
                        P.dma('sp', o_n[seq, d, h, dc * 128:(dc + 1) * 128].rearrange("(p o) -> p o", o=1), nst[:, 0:1], msem(), reads=[nst_b], is_output=True)
        nslot[0] = 6

    def dump(name):
        P.barrier()
        bufs = [xT_b[c][g] for c in range(NCH) for g in range(3)]
        P.dma('sp', dbg_out[name], fview(O_X, NCH * TT), msem(), reads=bufs, is_output=True)

    load_x()
    if 'l0mix' in S or 'l0ffn' in S:
        adaln(0, True)
    if 'l0mix' in S:
        l0_mixer(0, TP, False, 0)
        l0_mixer(TP, TP, False, 1)
        l0_mixer(2 * TP, TS, True, 0)
        if 'x_mix0' in dbg_out:
            dump('x_mix0')
    if 'l0ffn' in S:
        ffn_phase(0)
        if 'x_ffn0' in dbg_out:
            dump('x_ffn0')
    if 'l1mix' in S or 'l1ffn' in S:
        adaln(1, not ('l0mix' in S or 'l0ffn' in S))
    if 'l1mix' in S:
        l1_mixer(0, TP, False, 0)
        l1_mixer(TP, TP, False, 1)
        l1_mixer(2 * TP, TS, True, 0)
        if 'x_mix1' in dbg_out:
            dump('x_mix1')
    if 'l1ffn' in S:
        ffn_phase(1)
        if 'x_ffn1' in dbg_out:
            dump('x_ffn1')
    if 'final' in S:
        final_phase()
    stats = P.finalize()
    return nc, stats


def rope_tables():
    T = TS
    rows = T // 64
    row = np.repeat(np.arange(rows, dtype=np.float32), 64)
    col = np.tile(np.arange(64, dtype=np.float32), rows)
    half = 32
    inv = (10000.0 ** (-np.arange(0, half, 2, dtype=np.float32) / half)).astype(np.float32)
    ar = row[:, None] * inv
    ac = col[:, None] * inv
    cosT = np.zeros((64, T), np.float32)
    sinT = np.zeros((64, T), np.float32)
    cosT[0:16] = np.cos(ar).T
    cosT[16:32] = np.cos(ar).T
    cosT[32:48] = np.cos(ac).T
    cosT[48:64] = np.cos(ac).T
    sinT[0:16] = -np.sin(ar).T
    sinT[16:32] = np.sin(ar).T
    sinT[32:48] = -np.sin(ac).T
    sinT[48:64] = np.sin(ac).T
    return cosT, sinT


_CACHE = {}


def kernel(x_prompt, x_sample, c, cache_ckv, cache_krope, state_C, state_n, state_m,
           c_ctx, w_mod, b_mod, g_mix, g_ffn,
           w_in_a, g_q, g_kv, w_uq, w_ukv, conv_w, w_o_a,
           w_in_c, b_gates, g_h, w_o_c,
           w_ffn_gate, w_ffn_up, w_ffn_down,
           w_router, w_exp_gate, w_exp_up, w_exp_down, g_final):
    f = lambda a: np.ascontiguousarray(np.asarray(a), dtype=np.float32)
    ncores = 8
    if 'nc' not in _CACHE:
        _CACHE['nc'] = build()[0]
    nc = _CACHE['nc']
    cosT, sinT = rope_tables()
    shared = dict(
        w_mod=f(w_mod), b_mod=f(b_mod), g_mix=f(g_mix), g_ffn=f(g_ffn),
        w_in_a=f(w_in_a[0]), g_q=f(g_q[0]), g_kv=f(g_kv[0]), w_uq=f(w_uq[0]), w_ukv=f(w_ukv[0]), conv_w=f(conv_w[0]), w_o_a=f(w_o_a[0]),
        ropec=cosT, ropes=sinT,
        w_fg=f(w_ffn_gate[0]), w_fu=f(w_ffn_up[0]), w_fd=f(w_ffn_down[0]),
        w_in_c=f(w_in_c[0]), b_gates=f(b_gates).reshape(1, 16), g_h=f(g_h).reshape(1, D), w_o_c=f(w_o_c[0]),
        w_router=f(w_router[0]), w_eg=f(w_exp_gate[0]), w_eu=f(w_exp_up[0]), w_ed=f(w_exp_down[0]), g_final=f(g_final),
    )
    x_prompt = np.asarray(x_prompt)
    x_sample = np.asarray(x_sample)
    in_maps = []
    for i in range(ncores):
        m = dict(shared)
        m["xin"] = f(np.concatenate([x_prompt[2 * i:2 * i + 2].reshape(2 * TP, D), x_sample[i]], 0))
        m["cvec"] = f(np.stack([np.asarray(c_ctx), np.asarray(c)[i]], 0))
        m["cache_ckv"] = f(np.asarray(cache_ckv)[i, 0])
        m["cache_kr"] = f(np.asarray(cache_krope)[i, 0])
        m["st_C"] = f(np.asarray(state_C)[i, 0])
        m["st_n"] = f(np.asarray(state_n)[i, 0])
        m["st_m"] = f(np.asarray(state_m)[i, 0]).reshape(1, 8)
        in_maps.append(m)
    res = run_bass_kernel_spmd(nc, in_maps, core_ids=list(range(ncores)))
    R = res.results
    y_prompt = np.zeros((16, TP, D), np.float32)
    y_sample = np.zeros((8, TS, D), np.float32)
    n_ckv = np.zeros((16, 1, TP, 512), np.float32)
    n_kr = np.zeros((16, 1, TP, 64), np.float32)
    n_C = np.zeros((16, 1, 2, 4, 256, 512), np.float32)
    n_n = np.zeros((16, 1, 2, 4, 256), np.float32)
    n_m = np.zeros((16, 1, 2, 4), np.float32)
    for i in range(ncores):
        r = R[i]
        y_prompt[2 * i:2 * i + 2] = r["yout"][:2 * TP].reshape(2, TP, D)
        y_sample[i] = r["yout"][2 * TP:]
        n_ckv[2 * i:2 * i + 2, 0] = r["o_ckv"]
        n_kr[2 * i:2 * i + 2, 0] = r["o_kr"]
        n_C[2 * i:2 * i + 2, 0] = r["o_C"]
        n_n[2 * i:2 * i + 2, 0] = r["o_n"]
        n_m[2 * i:2 * i + 2, 0] = r["o_m"].reshape(2, 2, 4)
    return (y_prompt, y_sample, n_ckv, n_kr, n_C, n_n, n_m)
```

```python
import math
import numpy as np
import concourse.bass as bass
import concourse.mybir as mybir
from concourse.bass_utils import run_bass_kernel_spmd

F32 = mybir.dt.float32
BF16 = mybir.dt.bfloat16
ALU = mybir.AluOpType
AF = mybir.ActivationFunctionType
AX = mybir.AxisListType

D = 2048
NCH = 16
TP = 256
TS = 1024
TT = 1536
DFF = 7168
NFC = DFF // 128
NE = 8
EPS = 1e-6
C1, C2, C3, C4, C5 = 512, 1024, 1088, 2112, 3136
COMPUTE = ('pe', 'act', 'dve', 'pool')


class Buf:
    __slots__ = ('name', 'w', 'r')

    def __init__(self, name=''):
        self.name = name
        self.w = None
        self.r = []


class Ins:
    __slots__ = ('eng', 'fn', 'deps', 'flag', 'ms', 'is_dma', 'sem', 'val', 'line')

    def __init__(self, eng, fn, deps, is_dma=False, sem=None, val=0):
        import sys as _s
        fr = _s._getframe(2)
        ln = []
        while fr is not None and len(ln) < 4:
            ln.append(fr.f_lineno)
            fr = fr.f_back
        self.line = ln
        self.eng = eng
        self.fn = fn
        self.deps = deps
        self.flag = False
        self.ms = 0
        self.is_dma = is_dma
        self.sem = sem
        self.val = val


class DSem:
    def __init__(self, prog, name, serial=False):
        self.h = prog.nc.alloc_semaphore(name=name)
        self.count = 0
        self.serial = serial
        self.last = None


class Prog:
    def __init__(self, nc):
        self.nc = nc
        self.q = {e: [] for e in ('pe', 'act', 'dve', 'pool', 'sp')}
        self.msem = {}
        self.nsem = 0
        self.out_dmas = []
        self.fence = {}
        self.all_dsems = []

    def dsem(self, name=None, serial=False):
        self.nsem += 1
        d = DSem(self, name or f"ds{self.nsem}", serial)
        self.all_dsems.append(d)
        return d

    def barrier(self):
        deps = []
        for e, lst in self.q.items():
            for ins in reversed(lst):
                if not ins.is_dma:
                    deps.append(ins)
                    break
        for d in self.all_dsems:
            if d.last is not None:
                deps.append(d.last)
        for e in self.q:
            self.fence[e] = list(deps)

    def _fence(self, eng, deps):
        f = self.fence.pop(eng, None)
        if f:
            deps.extend(f)
        return deps

    def _deps(self, reads, writes, extra):
        deps = []
        for b in reads:
            if b.w is not None:
                deps.append(b.w)
        for b in writes:
            if b.w is not None:
                deps.append(b.w)
            deps.extend(b.r)
        if extra:
            deps.extend(extra)
        return deps

    def _commit(self, ins, reads, writes):
        for b in reads:
            b.r.append(ins)
            if len(b.r) > 48:
                last = {}
                keep = []
                for i in b.r:
                    if i.is_dma:
                        keep.append(i)
                    else:
                        last[i.eng] = i
                b.r = keep + list(last.values())
        for b in writes:
            b.w = ins
            b.r = []

    def op(self, eng, fn, reads=(), writes=(), extra=None):
        ins = Ins(eng, fn, self._fence(eng, self._deps(reads, writes, extra)))
        self.q[eng].append(ins)
        self._commit(ins, reads, writes)
        return ins

    def dma(self, eng, out, in_, sem, reads=(), writes=(), extra=None, is_output=False, **kw):
        deps = self._fence(eng, self._deps(reads, writes, extra))
        if sem.serial and sem.last is not None:
            deps.append(sem.last)
        sem.count += 16
        ins = Ins(eng, None, deps, is_dma=True, sem=sem, val=sem.count)
        sem.last = ins
        ins.fn = lambda e, out=out, in_=in_, kw=kw: e.dma_start(out=out, in_=in_, **kw)
        self.q[eng].append(ins)
        self._commit(ins, reads, writes)
        if is_output:
            self.out_dmas.append(ins)
        return ins

    def finalize(self):
        nc = self.nc
        for e, lst in self.q.items():
            for ins in lst:
                for d in ins.deps:
                    if not d.is_dma and not (d.eng == 'pe' and ins.eng == 'pe'):
                        d.flag = True
        for e in COMPUTE:
            n = 0
            for ins in self.q[e]:
                if not ins.is_dma and ins.flag:
                    n += 1
                    ins.ms = n
            self.msem[e] = nc.alloc_semaphore(name=f"ms_{e}")
        final = {}
        for d in self.out_dmas:
            k = id(d.sem)
            if k not in final or final[k][1] < d.val:
                final[k] = (d.sem, d.val)
        engmap = {'pe': 'tensor', 'act': 'scalar', 'dve': 'vector', 'pool': 'gpsimd', 'sp': 'sync'}
        stats = {}
        with nc.Block() as block:
            for e in ('sp', 'pool', 'act', 'dve', 'pe'):
                lst = self.q[e]

                def body(eng, e=e, lst=lst):
                    waited = {}
                    nw = 0
                    for ins in lst:
                        need = {}
                        for d in ins.deps:
                            if d.is_dma:
                                key = ('d', id(d.sem))
                                h = d.sem.h
                                v = d.val
                            else:
                                if d.eng == 'pe' and e == 'pe':
                                    continue
                                key = ('m', d.eng)
                                h = self.msem[d.eng]
                                v = d.ms
                            if key not in need or need[key][1] < v:
                                need[key] = (h, v)
                        for key, (h, v) in need.items():
                            if waited.get(key, 0) >= v:
                                continue
                            eng.wait_ge(h, v)
                            nw += 1
                            waited[key] = v
                        try:
                            bi = ins.fn(eng)
                        except BaseException as ex:
                            print("EMIT FAIL eng", e, "line", ins.line, "dma", ins.is_dma, repr(ex)[:2000], flush=True)
                            raise
                        if ins.is_dma:
                            bi.then_inc(ins.sem.h, 16)
                        elif ins.flag:
                            bi.then_inc(self.msem[e], 1)
                    if e == 'sp':
                        for (s, v) in final.values():
                            eng.wait_ge(s.h, v)
                    stats[e] = (len(lst), nw)
                getattr(block, engmap[e])(body)
        return stats


def build(stages=('l0mix', 'l0ffn', 'l1mix', 'l1ffn', 'final'), dbg=(), ne_run=NE, nfc_run=NFC):
    nc = bass.Bass("TRN2", target_bir_lowering=False)
    P = Prog(nc)
    dram = {}

    def din(name, shape):
        t = nc.dram_tensor(name, list(shape), F32, kind="ExternalInput").ap()
        dram[name] = t
        return t

    def dout(name, shape):
        return nc.dram_tensor(name, list(shape), F32, kind="ExternalOutput").ap()

    S = set(stages)
    xin = din("xin", [TT, D])
    cvec = din("cvec", [2, D])
    w_mod = din("w_mod", [2, D, 6 * D])
    b_mod = din("b_mod", [2, 6 * D])
    g_mix = din("g_mix", [2, D])
    g_ffn = din("g_ffn", [2, D])
    if 'l0mix' in S:
        cache_ckv = din("cache_ckv", [512, 512])
        cache_kr = din("cache_kr", [512, 64])
        w_in_a = din("w_in_a", [D, 4160])
        g_q = din("g_q", [512])
        g_kv = din("g_kv", [512])
        w_uq = din("w_uq", [512, 1536])
        w_ukv = din("w_ukv", [512, 2048])
        conv_w = din("conv_w", [3, 1024])
        w_o_a = din("w_o_a", [D, D])
        ropec = din("ropec", [64, TS])
        ropes = din("ropes", [64, TS])
        o_ckv = dout("o_ckv", [2, TP, 512])
        o_kr = dout("o_kr", [2, TP, 64])
    if 'l0ffn' in S:
        w_fg = din("w_fg", [D, DFF])
        w_fu = din("w_fu", [D, DFF])
        w_fd = din("w_fd", [DFF, D])
    if 'l1mix' in S:
        st_C = din("st_C", [2, 4, 256, 512])
        st_n = din("st_n", [2, 4, 256])
        st_m = din("st_m", [1, 8])
        w_in_c = din("w_in_c", [D, 6160])
        b_gates = din("b_gates", [1, 16])
        g_h = din("g_h", [1, D])
        w_o_c = din("w_o_c", [D, D])
        o_C = dout("o_C", [2, 2, 4, 256, 512])
        o_n = dout("o_n", [2, 2, 4, 256])
        o_m = dout("o_m", [2, 8])
    if 'l1ffn' in S:
        w_router = din("w_router", [D, NE])
        w_eg = din("w_eg", [ne_run, D, DFF])
        w_eu = din("w_eu", [ne_run, D, DFF])
        w_ed = din("w_ed", [ne_run, DFF, D])
    if 'final' in S:
        g_final = din("g_final", [D])
        yout = dout("yout", [TT, D])
    dbg_out = {}
    for name in dbg:
        if name in ('dg', 'lgt'):
            dbg_out[name] = dout("dbg_" + name, [128, 96])
        else:
            dbg_out[name] = dout("dbg_" + name, [128, NCH * TT])

    ARENA = 212800
    arena = nc.alloc_sbuf_tensor("arena", [128, ARENA // 4], F32).ap()

    def fview(off, n):
        assert off % 4 == 0 and off + 4 * n <= ARENA, (off, n)
        return arena[:, off // 4: off // 4 + n]

    def bview(off, n):
        assert off % 4 == 0 and n % 2 == 0 and off + 2 * n <= ARENA, (off, n)
        return arena[:, off // 4: off // 4 + n // 2].bitcast(BF16)

    O_X = 0
    MAXSLOT = 9
    nslot = [6]
    O_CONST = 98304
    O_MODV = O_CONST + 4096
    O_RING = O_MODV + 3072
    O_H = O_RING + 6 * 4096
    O_HF = O_RING + MAXSLOT * 4096
    SCR_END = ARENA

    xT = fview(O_X, NCH * TT).rearrange("p (c t) -> p c t", c=NCH)
    xT_b = [[Buf(f"x{c}_{g}") for g in range(3)] for c in range(NCH)]
    ring = [bview(O_RING + i * 4096, 2048) for i in range(MAXSLOT)]
    ring_b = [Buf(f"ring{i}") for i in range(MAXSLOT)]
    ring_s = [P.dsem(f"rs{i}") for i in range(MAXSLOT)]
    ring_i = [0]
    f32slab_s = [P.dsem(f"f32s{i}") for i in range(6)]

    ident_f = fview(O_CONST, 128)
    ident_b = bview(O_CONST + 512, 128)
    ones_b = bview(O_CONST + 768, 128)
    ones_f = fview(O_CONST + 1024, 128)
    triU = fview(O_CONST + 1536, 128)
    triL = fview(O_CONST + 2048, 128)
    negU = fview(O_CONST + 2560, 128)
    negL = fview(O_CONST + 3072, 128)
    cbuf = Buf("const")
    ps = [nc.alloc_psum_tensor(f"ps{i}", [128, 512], F32).ap() for i in range(8)]
    psb = [Buf(f"ps{i}") for i in range(8)]

    def PE(out, lhsT, rhs, start, stop, rd, wr, **kw):
        return P.op('pe', lambda e: e.matmul(out, lhsT, rhs, start=start, stop=stop, **kw), rd, wr)

    def TR(out, in_, idn, rd, wr):
        return P.op('pe', lambda e: e.transpose(out, in_, idn), rd, wr)

    def ACT(out, in_, func, rd, wr, **kw):
        return P.op('act', lambda e: e.activation(out=out, in_=in_, func=func, **kw), rd, wr)

    def V(eng, method, rd, wr, *a, **kw):
        return P.op(eng, lambda e: getattr(e, method)(*a, **kw), rd, wr)

    def DVE(method, rd, wr, *a, **kw):
        return V('dve', method, rd, wr, *a, **kw)

    def POOL(method, rd, wr, *a, **kw):
        return V('pool', method, rd, wr, *a, **kw)

    def slab(dst_fn, src):
        i = ring_i[0] % nslot[0]
        ring_i[0] += 1
        v = dst_fn(ring[i])
        P.dma('pool', v, src, ring_s[i], writes=[ring_b[i]])
        return v, ring_b[i]

    def kslab(w, c0, n):
        return slab(lambda r: r[:, 0:16 * n].rearrange("p (k c) -> p k c", k=16),
                    w[:, c0:c0 + n].rearrange("(k p) c -> p k c", p=128))

    POOL('memset', [], [cbuf], ident_f, 0.0)
    POOL('affine_select', [], [cbuf], out=ident_f, in_=ident_f, pattern=[[-1, 128]], compare_op=ALU.not_equal,
         fill=1.0, base=0, channel_multiplier=1)
    POOL('tensor_copy', [], [cbuf], out=ident_b, in_=ident_f)
    POOL('memset', [], [cbuf], ones_b, 1.0)
    POOL('memset', [], [cbuf], ones_f, 1.0)
    POOL('memset', [], [cbuf], triU, 1.0)
    POOL('affine_select', [], [cbuf], out=triU, in_=triU, pattern=[[1, 128]], compare_op=ALU.is_ge,
         fill=0.0, base=0, channel_multiplier=-1)
    POOL('memset', [], [cbuf], triL, 1.0)
    POOL('affine_select', [], [cbuf], out=triL, in_=triL, pattern=[[-1, 128]], compare_op=ALU.is_ge,
         fill=0.0, base=0, channel_multiplier=1)

    POOL('memset', [], [cbuf], negU, 0.0)
    POOL('affine_select', [], [cbuf], out=negU, in_=negU, pattern=[[1, 128]], compare_op=ALU.is_ge,
         fill=-30000.0, base=0, channel_multiplier=-1)
    POOL('memset', [], [cbuf], negL, 0.0)
    POOL('affine_select', [], [cbuf], out=negL, in_=negL, pattern=[[-1, 128]], compare_op=ALU.is_ge,
         fill=-30000.0, base=0, channel_multiplier=1)

    misc_s = [P.dsem(f"misc{i}", serial=True) for i in range(4)]
    misc_i = [0]

    def msem():
        misc_i[0] += 1
        return misc_s[misc_i[0] % 4]

    pmisc_s = [P.dsem(f"pmisc{i}", serial=True) for i in range(2)]
    pmisc_i = [0]

    def pmsem():
        pmisc_i[0] += 1
        return pmisc_s[pmisc_i[0] % 2]

    class Scr:
        def __init__(self, lo, hi):
            self.lo, self.hi, self.p = lo, hi, lo

        def ft(self, n):
            self.hi -= 4 * n
            assert self.p <= self.hi, ("scratch overflow", self.p, self.hi)
            return fview(self.hi, n)

        def bt(self, n):
            n2 = (n + 1) // 2 * 2
            self.hi -= 2 * n2
            assert self.p <= self.hi, ("scratch overflow", self.p, self.hi)
            return bview(self.hi, n2)[:, 0:n]

        def f(self, n):
            o = self.p
            self.p += 4 * n
            assert self.p <= self.hi, ("scratch overflow", self.p, self.hi)
            return fview(o, n)

        def b(self, n):
            n2 = (n + 1) // 2 * 2
            o = self.p
            self.p += 2 * n2
            assert self.p <= self.hi, ("scratch overflow", self.p, self.hi)
            return bview(o, n2)[:, 0:n]

    phase_buf = Buf("phase")

    def load_cols(dst, vec_rows, n, tmp):
        tmp_rows, tb = tmp
        P.dma('sp', tmp_rows[0:n, :], vec_rows, msem(), writes=[tb])
        TR(ps[7][:, 0:n], tmp_rows[0:n, :], ident_f[0:n, 0:n], [tb, cbuf], [psb[7]])
        b = Buf()
        DVE('tensor_copy', [], [psb[7], b], out=dst, in_=ps[7][:, 0:n])
        return b

    def load_x():
        P.barrier()
        sc = Scr(O_H, ARENA)
        stg = [sc.f(2048) for _ in range(2)]
        stg_b = [Buf() for _ in range(2)]
        stg_s = [P.dsem() for _ in range(2)]
        for t in range(TT // 128):
            s = t % 2
            g = t // 4
            P.dma('sp', stg[s], xin[t * 128:(t + 1) * 128, :], stg_s[s], writes=[stg_b[s]])
            for c4 in range(4):
                bk = (t * 4 + c4) % 8
                for j in range(4):
                    c = c4 * 4 + j
                    TR(ps[bk][:, j * 128:(j + 1) * 128], stg[s][:, c * 128:(c + 1) * 128], ident_f, [stg_b[s], cbuf], [psb[bk]])
                wr = [psb[bk]] + [xT_b[c4 * 4 + j][g] for j in range(4)]
                o = xT[:, c4 * 4:c4 * 4 + 4, t * 128:(t + 1) * 128]
                i = ps[bk].rearrange("p (j t) -> p j t", j=4)
                if c4 % 2 == 0:
                    DVE('tensor_copy', [], wr, out=o, in_=i)
                else:
                    P.op('act', lambda e, o=o, i=i: e.copy(out=o, in_=i), [], wr)

    modT = fview(O_MODV, 192).rearrange("p (f b) -> p f b", b=2)
    AB = fview(O_MODV + 768, 6 * 32).rearrange("p (m c b) -> p m c b", m=6, c=16)
    gv = fview(O_MODV + 768 + 768, 3 * 16).rearrange("p (m c) -> p m c", m=3)
    condT = fview(O_MODV + 768 + 768 + 192, 32).rearrange("p (b k) -> p b k", b=2)
    mod_b = Buf("mod")

    def adaln(l, first):
        P.barrier()
        sc = Scr(O_H, ARENA)
        tmpr = (sc.f(128), Buf())
        ctmp = sc.f(32)
        bm = sc.f(96)
        if first:
            cb = load_cols(ctmp, cvec.rearrange("b (k p) -> (b k) p", p=128), 32, tmpr)
            ACT(condT, ctmp.rearrange("p (b k) -> p b k", b=2), AF.Silu, [cb], [mod_b])
        bmb = load_cols(bm, b_mod[l].rearrange("(f p) -> f p", p=128), 96, tmpr)
        g1b = load_cols(gv[:, 0, :], g_mix[l].rearrange("(f p) -> f p", p=128), 16, tmpr)
        g2b = load_cols(gv[:, 1, :], g_ffn[l].rearrange("(f p) -> f p", p=128), 16, tmpr)
        rowb = [sc.f(512) for _ in range(2)]
        rowb_b = [Buf() for _ in range(2)]
        si = 0
        for cg in range(24):
            bk = cg % 2
            for k2 in range(8):
                j = si % 6
                si += 1
                wv = fview(O_RING + j * 4096, 1024).rearrange("p (kk c) -> p kk c", kk=2)
                P.dma('sp', wv, w_mod[l][k2 * 256:(k2 + 1) * 256, cg * 512:(cg + 1) * 512].rearrange("(kk p) c -> p kk c", p=128),
                      f32slab_s[j], writes=[ring_b[j]])
                for kk in range(2):
                    k = k2 * 2 + kk
                    PE(ps[bk][0:2, :], condT[:, :, k], wv[:, kk, :], k == 0, k == 15, [ring_b[j], mod_b], [psb[bk]])
            DVE('tensor_copy', [], [psb[bk], rowb_b[bk]], out=rowb[bk][0:2, :], in_=ps[bk][0:2, :])
            for jj in range(4):
                f = cg * 4 + jj
                TR(ps[6][:, 2 * f:2 * f + 2], rowb[bk][0:2, jj * 128:(jj + 1) * 128], ident_f[0:2, 0:2], [rowb_b[bk], cbuf], [psb[6]])
        DVE('tensor_tensor', [bmb], [psb[6], mod_b], out=modT, in0=ps[6][:, 0:192].rearrange("p (f b) -> p f b", b=2),
            in1=bm.unsqueeze(2).to_broadcast([128, 96, 2]), op=ALU.add)
        for b in range(2):
            DVE('scalar_tensor_tensor', [g1b], [mod_b], out=AB[:, 0, :, b], in0=modT[:, 16:32, b], scalar=1.0, in1=gv[:, 0, :],
                op0=ALU.add, op1=ALU.mult)
            DVE('tensor_copy', [], [mod_b], out=AB[:, 1, :, b], in_=modT[:, 0:16, b])
            DVE('tensor_copy', [], [mod_b], out=AB[:, 2, :, b], in_=modT[:, 32:48, b])
            DVE('scalar_tensor_tensor', [g2b], [mod_b], out=AB[:, 3, :, b], in0=modT[:, 64:80, b], scalar=1.0, in1=gv[:, 1, :],
                op0=ALU.add, op1=ALU.mult)
            DVE('tensor_copy', [], [mod_b], out=AB[:, 4, :, b], in_=modT[:, 48:64, b])
            DVE('tensor_copy', [], [mod_b], out=AB[:, 5, :, b], in_=modT[:, 80:96, b])

    def grp_of(tok):
        return tok // 512

    def modulate(hT, hT_b, tok0, T, ia, ib, bsel, sc, side=None):
        P.barrier()
        NG = max(1, T // 512)
        N = min(512, T)
        sq = [sc.b(512) for _ in range(2)]
        sq_b = [Buf() for _ in range(2)]
        rstd = sc.f(512)
        rstd_b = Buf()
        tmp = [sc.f(512) for _ in range(2)]
        tmp_b = [Buf() for _ in range(2)]
        if side is not None:
            sqf = [sc.f(512) for _ in range(2)]
        if side is not None and len(side) > 4:
            hs2 = [sc.b(512) for _ in range(2)]
            hs3 = [sc.b(512) for _ in range(2)]
            hs2_b = [Buf() for _ in range(2)]
            hs3_b = [Buf() for _ in range(2)]
        for gi in range(NG):
            t0 = tok0 + gi * N
            g = grp_of(t0)
            for c in range(NCH):
                q = c % 2
                if side is not None:
                    ACT(sqf[q][:, 0:N], xT[:, c, t0:t0 + N], AF.Square, [xT_b[c][g]], [sq_b[q]])
                    PE(ps[7][:, 0:N], ones_f, sqf[q][:, 0:N], c == 0, c == NCH - 1, [sq_b[q], cbuf], [psb[7]])
                else:
                    ACT(sq[q][:, 0:N], xT[:, c, t0:t0 + N], AF.Square, [xT_b[c][g]], [sq_b[q]])
                    PE(ps[7][:, 0:N], ones_b, sq[q][:, 0:N], c == 0, c == NCH - 1, [sq_b[q], cbuf], [psb[7]])
            if side is not None:
                vms = tmp[0]
                DVE('tensor_scalar', [], [psb[7], tmp_b[0]], out=vms[:, 0:N], in0=ps[7][:, 0:N], scalar1=1.0 / D, scalar2=EPS,
                    op0=ALU.mult, op1=ALU.add)
                P.op('act', lambda e, o=rstd[:, 0:N], i=vms[:, 0:N]: e.sqrt(out=o, in_=i), [tmp_b[0]], [rstd_b])
                DVE('reciprocal', [], [rstd_b], out=rstd[:, 0:N], in_=rstd[:, 0:N])
                DVE('tensor_tensor', [rstd_b], [tmp_b[0]], out=vms[:, 0:N], in0=vms[:, 0:N], in1=rstd[:, 0:N], op=ALU.mult)
                DVE('tensor_tensor', [rstd_b], [tmp_b[0]], out=vms[:, 0:N], in0=vms[:, 0:N], in1=rstd[:, 0:N], op=ALU.mult)
                DVE('tensor_scalar', [], [tmp_b[0]], out=vms[:, 0:N], in0=vms[:, 0:N], scalar1=-0.5, scalar2=1.5, op0=ALU.mult, op1=ALU.add)
                DVE('tensor_tensor', [tmp_b[0]], [rstd_b], out=rstd[:, 0:N], in0=rstd[:, 0:N], in1=vms[:, 0:N], op=ALU.mult)
            else:
                DVE('tensor_scalar', [], [psb[7], rstd_b], out=rstd[:, 0:N], in0=ps[7][:, 0:N], scalar1=1.0 / D, scalar2=EPS,
                    op0=ALU.mult, op1=ALU.add)
                P.op('act', lambda e, o=rstd[:, 0:N], i=rstd[:, 0:N]: e.sqrt(out=o, in_=i), [], [rstd_b])
                DVE('reciprocal', [], [rstd_b], out=rstd[:, 0:N], in_=rstd[:, 0:N])
            for c in range(NCH):
                q = c % 2
                DVE('scalar_tensor_tensor', [xT_b[c][g], rstd_b, mod_b], [tmp_b[q]], out=tmp[q][:, 0:N], in0=xT[:, c, t0:t0 + N],
                    scalar=AB[:, ia, c, bsel:bsel + 1], in1=rstd[:, 0:N], op0=ALU.mult, op1=ALU.mult)
                if side is not None:
                    w32, n, bank, wbuf = side[:4]
                    DVE('tensor_scalar', [mod_b], [tmp_b[q]], out=tmp[q][:, 0:N], in0=tmp[q][:, 0:N],
                        scalar1=AB[:, ib, c, bsel:bsel + 1], scalar2=None, op0=ALU.add)
                    P.op('act', lambda e, o=hT[:, c, gi * N:gi * N + N], i=tmp[q][:, 0:N]: e.copy(out=o, in_=i), [tmp_b[q]], [hT_b[c][gi]])
                    if len(side) > 4:
                        w1, w2, w3 = side[4]
                        h1 = hT[:, c, gi * N:gi * N + N]
                        DVE('tensor_tensor', [hT_b[c][gi]], [tmp_b[q]], out=tmp[q][:, 0:N], in0=tmp[q][:, 0:N], in1=h1, op=ALU.subtract)
                        P.op('act', lambda e, o=hs2[q][:, 0:N], i=tmp[q][:, 0:N]: e.copy(out=o, in_=i), [tmp_b[q]], [hs2_b[q]])
                        DVE('tensor_tensor', [hs2_b[q]], [tmp_b[q]], out=tmp[q][:, 0:N], in0=tmp[q][:, 0:N], in1=hs2[q][:, 0:N], op=ALU.subtract)
                        P.op('act', lambda e, o=hs3[q][:, 0:N], i=tmp[q][:, 0:N]: e.copy(out=o, in_=i), [tmp_b[q]], [hs3_b[q]])
                        terms = [(h1, [hT_b[c][gi]], 0, w1), (h1, [hT_b[c][gi]], 0, w2), (hs2[q], [hs2_b[q]], 1, w1), (hs2[q], [hs2_b[q]], 1, w2),
                                 (h1, [hT_b[c][gi]], 0, w3), (hs3[q], [hs3_b[q]], 1, w1)]
                        for tl in range(N // 128):
                            tile = gi * (N // 128) + tl
                            for ti, (ha, hab, loc, wa) in enumerate(terms):
                                PE(ps[bank][:, tile * n:(tile + 1) * n], ha[:, tl * 128:(tl + 1) * 128], wa[:, c, :],
                                   (c == 0 and tl == 0 and gi == 0 and ti == 0), (c == NCH - 1 and ti == len(terms) - 1), hab + [wbuf], [psb[bank]],
                                   skip_group_check=True)
                    else:
                        for tl in range(N // 128):
                            tile = gi * (N // 128) + tl
                            PE(ps[bank][:, tile * n:(tile + 1) * n], tmp[q][:, tl * 128:(tl + 1) * 128], w32[:, c, :],
                               (c == 0 and tl == 0 and gi == 0), (c == NCH - 1), [tmp_b[q], wbuf], [psb[bank]], skip_group_check=True)
                else:
                    ACT(hT[:, c, gi * N:gi * N + N], tmp[q][:, 0:N], AF.Identity, [tmp_b[q], mod_b], [hT_b[c][gi]],
                        bias=AB[:, ib, c, bsel:bsel + 1], scale=1.0)

    def resid_add(bank, n0, N, c, tok, ig, bsel):
        g = grp_of(tok)
        DVE('scalar_tensor_tensor', [mod_b], [psb[bank], xT_b[c][g]], out=xT[:, c, tok:tok + N], in0=ps[bank][:, n0:n0 + N],
            scalar=AB[:, ig, c, bsel:bsel + 1], in1=xT[:, c, tok:tok + N], op0=ALU.mult, op1=ALU.add)

    def out_proj(w_o, krow0, nk, rhsT, rhs_bufs, tok0, T, ig, bsel, banks=(4, 5)):
        N = min(512, T)
        slabs = [slab(lambda r: r, w_o[(krow0 + k) * 128:(krow0 + k + 1) * 128, :]) for k in range(nk)]
        i = 0
        for m in range(NCH):
            for gi in range(T // N):
                bk = banks[i % len(banks)]
                i += 1
                for k in range(nk):
                    wv, wb = slabs[k]
                    PE(ps[bk][:, 0:N], wv[:, m * 128:(m + 1) * 128], rhsT[:, k, gi * N:gi * N + N], k == 0, k == nk - 1,
                       [wb] + rhs_bufs, [psb[bk]])
                resid_add(bk, 0, N, m, tok0 + gi * N, ig, bsel)

    def l0_mixer(tok0, T, sample, seq):
        P.barrier()
        bsel = 1 if sample else 0
        N = min(512, T)
        NG = T // N
        Tk = T + (512 if sample else 0)
        R = Scr(O_H, ARENA)
        hT = R.b(NCH * T).rearrange("p (c t) -> p c t", c=NCH)
        hT_b = [[Buf() for _ in range(NG)] for _ in range(NCH)]
        cqn = R.bt(4 * T).rearrange("p (c t) -> p c t", c=4)
        ckvn = R.bt(4 * Tk).rearrange("p (c t) -> p c t", c=4)
        krT = R.bt(Tk)
        cq_b, ckv_b, kr_b = Buf(), Buf(), Buf()
        gq = R.ft(4)
        gkv = R.ft(4)
        cw = R.ft(24).rearrange("p (j c) -> p j c", j=3)
        tmpr = (R.ft(128), Buf())
        gq_b = load_cols(gq, g_q.rearrange("(f p) -> f p", p=128), 4, tmpr)
        gkv_b = load_cols(gkv, g_kv.rearrange("(f p) -> f p", p=128), 4, tmpr)
        cw_b = [load_cols(cw[:, j, :], conv_w[j].rearrange("(f p) -> f p", p=128), 8, tmpr) for j in range(3)]
        if sample:
            cosT = R.bt(TS)
            sinT = R.bt(TS)
            rope_b = Buf()
            P.dma('pool', cosT[0:64, :], ropec, pmsem(), writes=[rope_b])
            P.dma('pool', sinT[0:64, :], ropes, pmsem(), writes=[rope_b])
        mark = R.p
        SCR_HI = R.hi
        scm = Scr(mark, SCR_HI)
        modulate(hT, hT_b, tok0, T, 0, 1, bsel, scm)

        P.barrier()
        sc3 = Scr(mark, SCR_HI)
        sq = [sc3.b(512) for _ in range(2)]
        sq_b = [Buf() for _ in range(2)]
        rstd = sc3.f(512)
        rstd_b = Buf()
        f32o = sc3.f(512)
        f32o_b = Buf()
        ostg = [sc3.f(512) for _ in range(2)]
        ostg_b = [Buf() for _ in range(2)]
        ostg_s = [P.dsem() for _ in range(2)]
        oi = [0]
        for (col0, gvec, gb, dst, dst_b, is_kv) in ((0, gq, gq_b, cqn, cq_b, False), (C1, gkv, gkv_b, ckvn, ckv_b, True)):
            for gi in range(NG):
                for c in range(4):
                    wv, wb = kslab(w_in_a, col0 + c * 128, 128)
                    for k in range(NCH):
                        PE(ps[c][:, 0:N], wv[:, k, :], hT[:, k, gi * N:gi * N + N], k == 0, k == NCH - 1, [wb, hT_b[k][gi]], [psb[c]])
                    q = c % 2
                    ACT(sq[q][:, 0:N], ps[c][:, 0:N], AF.Square, [], [psb[c], sq_b[q]])
                    PE(ps[7][:, 0:N], ones_b, sq[q][:, 0:N], c == 0, c == 3, [sq_b[q], cbuf], [psb[7]])
                DVE('tensor_scalar', [], [psb[7], rstd_b], out=rstd[:, 0:N], in0=ps[7][:, 0:N], scalar1=1.0 / 512, scalar2=EPS,
                    op0=ALU.mult, op1=ALU.add)
                P.op('act', lambda e, o=rstd[:, 0:N], i=rstd[:, 0:N]: e.sqrt(out=o, in_=i), [], [rstd_b])
                DVE('reciprocal', [], [rstd_b], out=rstd[:, 0:N], in_=rstd[:, 0:N])
                for c in range(4):
                    if is_kv and not sample:
                        DVE('scalar_tensor_tensor', [gb, rstd_b], [psb[c], f32o_b], out=f32o[:, 0:N], in0=ps[c][:, 0:N],
                            scalar=gvec[:, c:c + 1], in1=rstd[:, 0:N], op0=ALU.mult, op1=ALU.mult)
                        P.op('act', lambda e, o=dst[:, c, gi * N:gi * N + N], i=f32o[:, 0:N]: e.copy(out=o, in_=i), [f32o_b], [dst_b])
                        for tl in range(N // 128):
                            TR(ps[4 + tl][:, c * 128:(c + 1) * 128], f32o[:, tl * 128:(tl + 1) * 128], ident_f, [f32o_b, cbuf], [psb[4 + tl]])
                    else:
                        DVE('scalar_tensor_tensor', [gb, rstd_b], [psb[c], dst_b], out=dst[:, c, gi * N:gi * N + N], in0=ps[c][:, 0:N],
                            scalar=gvec[:, c:c + 1], in1=rstd[:, 0:N], op0=ALU.mult, op1=ALU.mult)
                if is_kv and not sample:
                    for tl in range(N // 128):
                        s = oi[0] % 2
                        oi[0] += 1
                        DVE('tensor_copy', [], [psb[4 + tl], ostg_b[s]], out=ostg[s], in_=ps[4 + tl])
                        P.dma('sp', o_ckv[seq, gi * N + tl * 128: gi * N + (tl + 1) * 128, :], ostg[s], ostg_s[s], reads=[ostg_b[s]], is_output=True)
        krf = sc3.f(512)
        krf_b = Buf()
        krf2 = sc3.f(512)
        krf2_b = Buf()
        for gi in range(NG):
            wv, wb = kslab(w_in_a, C2, 64)
            for k in range(NCH):
                PE(ps[0][0:64, 0:N], wv[:, k, :], hT[:, k, gi * N:gi * N + N], k == 0, k == NCH - 1, [wb, hT_b[k][gi]], [psb[0]])
            if sample:
                i = ring_i[0] % nslot[0]
                ring_i[0] += 1
                wp = ring[i][:, 0:1024].rearrange("p (k c) -> p k c", k=16)
                wsrc = w_in_a[:, C2:C2 + 64].rearrange("(k p) c -> p k c", p=128)
                for a in range(2):
                    for b in range(2):
                        P.dma('pool', wp[:, :, a * 32 + b * 16: a * 32 + b * 16 + 16], wsrc[:, :, a * 32 + (1 - b) * 16: a * 32 + (1 - b) * 16 + 16],
                              ring_s[i], writes=[ring_b[i]])
                wpb = ring_b[i]
                for k in range(NCH):
                    PE(ps[1][0:64, 0:N], wp[:, k, :], hT[:, k, gi * N:gi * N + N], k == 0, k == NCH - 1, [wpb, hT_b[k][gi]], [psb[1]])
                DVE('tensor_tensor', [rope_b], [psb[0], krf_b], out=krf[0:64, 0:N], in0=ps[0][0:64, 0:N], in1=cosT[0:64, gi * N:gi * N + N], op=ALU.mult)
                DVE('tensor_tensor', [rope_b], [psb[1], krf2_b], out=krf2[0:64, 0:N], in0=ps[1][0:64, 0:N], in1=sinT[0:64, gi * N:gi * N + N], op=ALU.mult)
                POOL('tensor_tensor', [krf_b, krf2_b], [kr_b], out=krT[0:64, gi * N:gi * N + N], in0=krf[0:64, 0:N], in1=krf2[0:64, 0:N], op=ALU.add)
            else:
                DVE('tensor_copy', [], [psb[0], krf_b], out=krf[0:64, 0:N], in_=ps[0][0:64, 0:N])
                P.op('act', lambda e, o=krT[0:64, gi * N:gi * N + N], i=krf[0:64, 0:N]: e.copy(out=o, in_=i), [krf_b], [kr_b])
                for tl in range(N // 128):
                    TR(ps[2][:, tl * 64:(tl + 1) * 64], krf[0:64, tl * 128:(tl + 1) * 128], ident_f[0:64, 0:64], [krf_b, cbuf], [psb[2]])
                ob = Buf()
                DVE('tensor_copy', [], [psb[2], ob], out=krf2[:, 0:(N // 128) * 64], in_=ps[2][:, 0:(N // 128) * 64])
                for tl in range(N // 128):
                    P.dma('sp', o_kr[seq, gi * N + tl * 128: gi * N + (tl + 1) * 128, :], krf2[:, tl * 64:(tl + 1) * 64], msem(), reads=[ob], is_output=True)
        if sample:
            cst = [sc3.f(512) for _ in range(2)]
            cst_b = [Buf() for _ in range(2)]
            cst_s = [P.dsem() for _ in range(2)]
            for tl in range(4):
                s = tl % 2
                P.dma('sp', cst[s], cache_ckv[tl * 128:(tl + 1) * 128, :], cst_s[s], writes=[cst_b[s]])
                for c in range(4):
                    TR(ps[2][:, c * 128:(c + 1) * 128], cst[s][:, c * 128:(c + 1) * 128], ident_f, [cst_b[s], cbuf], [psb[2]])
                DVE('tensor_copy', [], [psb[2], ckv_b], out=ckvn[:, :, T + tl * 128:T + (tl + 1) * 128],
                    in_=ps[2].rearrange("p (c t) -> p c t", c=4))
            kb = Buf()
            P.dma('sp', krf[:, 0:256].rearrange("p (j d) -> p j d", j=4), cache_kr.rearrange("(j p) d -> p j d", p=128), msem(), writes=[krf_b, kb])
            for tl in range(4):
                TR(ps[3][0:64, tl * 128:(tl + 1) * 128], krf[:, tl * 64:(tl + 1) * 64], ident_f, [krf_b, cbuf], [psb[3]])
            DVE('tensor_copy', [], [psb[3], kr_b], out=krT[0:64, T:T + 512], in_=ps[3][0:64, :])

        P.barrier()
        sc4 = Scr(mark, SCR_HI)
        ppad = sc4.f(T + 2)
        ppad_b = Buf()
        ubs = sc4.f(T)
        ubs_b = Buf()
        uxs = sc4.f(512)
        uxs_b = Buf()
        oc = sc4.f(T)
        oc_b = Buf()
        mixc = sc4.b(T).rearrange("p (k t) -> p k t", k=1)
        mixc_b = Buf()
        POOL('memset', [], [ppad_b], ppad[:, 0:1], 0.0)
        POOL('memset', [], [ppad_b], ppad[:, T + 1:T + 2], 0.0)
        for c in range(8):
            for gi in range(NG):
                cset = (0, 1, 2) if (c * NG + gi) % 2 == 0 else (3, 6, 7)
                for j, (col0, bk) in enumerate(((C3, cset[0]), (C4, cset[1]), (C5, cset[2]))):
                    wv, wb = kslab(w_in_a, col0 + c * 128, 128)
                    for k in range(NCH):
                        PE(ps[bk][:, 0:N], wv[:, k, :], hT[:, k, gi * N:gi * N + N], k == 0, k == NCH - 1, [wb, hT_b[k][gi]], [psb[bk]])
                P.op('act', lambda e, o=uxs[:, 0:N], i=ps[cset[0]][:, 0:N]: e.copy(out=o, in_=i), [], [psb[cset[0]], uxs_b])
                P.op('act', lambda e, o=ubs[:, gi * N:gi * N + N], i=ps[cset[1]][:, 0:N]: e.copy(out=o, in_=i), [], [psb[cset[1]], ubs_b])
                DVE('tensor_tensor', [uxs_b], [psb[cset[2]], ppad_b], out=ppad[:, 1 + gi * N:1 + gi * N + N], in0=ps[cset[2]][:, 0:N], in1=uxs[:, 0:N], op=ALU.mult)
            DVE('tensor_scalar', [ppad_b, cw_b[0]], [oc_b], out=oc, in0=ppad[:, 0:T], scalar1=cw[:, 0, c:c + 1], scalar2=None, op0=ALU.mult)
            DVE('scalar_tensor_tensor', [ppad_b, cw_b[1]], [oc_b], out=oc, in0=ppad[:, 1:T + 1], scalar=cw[:, 1, c:c + 1], in1=oc, op0=ALU.mult, op1=ALU.add)
            DVE('scalar_tensor_tensor', [ppad_b, cw_b[2]], [oc_b], out=oc, in0=ppad[:, 2:T + 2], scalar=cw[:, 2, c:c + 1], in1=oc, op0=ALU.mult, op1=ALU.add)
            POOL('tensor_tensor', [oc_b, ubs_b], [mixc_b], out=mixc[:, 0, :], in0=oc, in1=ubs, op=ALU.mult)
            out_proj(w_o_a, 8 + c, 1, mixc, [mixc_b], tok0, T, 2, bsel, banks=(4, 5))

        P.barrier()
        sc5 = Scr(O_H, SCR_HI)
        qn = sc5.b(T)
        qr = sc5.b(T)
        kn = sc5.b(Tk)
        NKT = Tk // 128
        vt = sc5.b(NKT * 128).rearrange("p (j e) -> p j e", e=128)
        pT = [sc5.b(512) for _ in range(2)]
        pT_b = [Buf() for _ in range(2)]
        rden = sc5.f(512)
        rden_b = Buf()
        atth = sc5.b(T).rearrange("p (k t) -> p k t", k=1)
        atth_b = Buf()
        qt1 = sc5.f(512)
        qt1_b = Buf()
        qt2 = sc5.f(512)
        qt2_b = Buf()
        qn_b, qr_b, kn_b, vt_b = Buf(), Buf(), Buf(), Buf()
        scale = 192.0 ** -0.5
        for h in range(8):
            i = ring_i[0] % nslot[0]
            ring_i[0] += 1
            wq = ring[i][:, 0:1024].rearrange("p (k c) -> p k c", k=4)
            wqs = w_uq[:, h * 192:(h + 1) * 192].rearrange("(k p) c -> p k c", p=128)
            P.dma('pool', wq[:, :, 0:192], wqs, ring_s[i], writes=[ring_b[i]])
            if sample:
                for a in range(2):
                    for b in range(2):
                        P.dma('pool', wq[:, :, 192 + a * 32 + b * 16:192 + a * 32 + b * 16 + 16],
                              wqs[:, :, 128 + a * 32 + (1 - b) * 16:128 + a * 32 + (1 - b) * 16 + 16], ring_s[i], writes=[ring_b[i]])
            wq_b = ring_b[i]
            wkv, wkv_b = slab(lambda r: r[:, 0:1024].rearrange("p (k c) -> p k c", k=4),
                              w_ukv[:, h * 256:(h + 1) * 256].rearrange("(k p) c -> p k c", p=128))
            for gi in range(NG):
                for k in range(4):
                    PE(ps[0][:, 0:N], wq[:, k, 0:128], cqn[:, k, gi * N:gi * N + N], k == 0, k == 3, [wq_b, cq_b], [psb[0]])
                P.op('act', lambda e, o=qn[:, gi * N:gi * N + N], i=ps[0][:, 0:N]: e.copy(out=o, in_=i), [], [psb[0], qn_b])
                for k in range(4):
                    PE(ps[1][0:64, 0:N], wq[:, k, 128:192], cqn[:, k, gi * N:gi * N + N], k == 0, k == 3, [wq_b, cq_b], [psb[1]])
                if sample:
                    for k in range(4):
                        PE(ps[2][0:64, 0:N], wq[:, k, 192:256], cqn[:, k, gi * N:gi * N + N], k == 0, k == 3, [wq_b, cq_b], [psb[2]])
                    DVE('tensor_tensor', [rope_b], [psb[1], qt1_b], out=qt1[0:64, 0:N], in0=ps[1][0:64, 0:N], in1=cosT[0:64, gi * N:gi * N + N], op=ALU.mult)
                    DVE('tensor_tensor', [rope_b], [psb[2], qt2_b], out=qt2[0:64, 0:N], in0=ps[2][0:64, 0:N], in1=sinT[0:64, gi * N:gi * N + N], op=ALU.mult)
                    POOL('tensor_tensor', [qt1_b, qt2_b], [qr_b], out=qr[0:64, gi * N:gi * N + N], in0=qt1[0:64, 0:N], in1=qt2[0:64, 0:N], op=ALU.add)
                else:
                    P.op('act', lambda e, o=qr[0:64, gi * N:gi * N + N], i=ps[1][0:64, 0:N]: e.copy(out=o, in_=i), [], [psb[1], qr_b])
            for kg in range((Tk + 511) // 512):
                n = min(512, Tk - kg * 512)
                kb_ = 3 if kg % 2 == 0 else 4
                for k in range(4):
                    PE(ps[kb_][:, 0:n], wkv[:, k, 0:128], ckvn[:, k, kg * 512:kg * 512 + n], k == 0, k == 3, [wkv_b, ckv_b], [psb[kb_]])
                P.op('act', lambda e, o=kn[:, kg * 512:kg * 512 + n], i=ps[kb_][:, 0:n]: e.copy(out=o, in_=i), [], [psb[kb_], kn_b])
            for j0 in range(0, NKT, 4):
                nj = min(4, NKT - j0)
                vb_ = 2 if (j0 // 4) % 2 == 0 else 5
                for jj in range(nj):
                    for k in range(4):
                        PE(ps[vb_][:, jj * 128:(jj + 1) * 128], ckvn[:, k, (j0 + jj) * 128:(j0 + jj + 1) * 128], wkv[:, k, 128:256], k == 0, k == 3,
                           [wkv_b, ckv_b], [psb[vb_]])
                DVE('tensor_copy', [], [psb[vb_], vt_b], out=vt[:, j0:j0 + nj, :], in_=ps[vb_][:, 0:nj * 128].rearrange("p (j e) -> p j e", e=128))
            for gi in range(NG):
                def l0_scores(kt):
                    sb = kt % 2
                    PE(ps[sb][:, 0:N], kn[:, kt * 128:(kt + 1) * 128], qn[:, gi * N:gi * N + N], True, False, [kn_b, qn_b], [psb[sb]])
                    PE(ps[sb][:, 0:N], krT[0:64, kt * 128:(kt + 1) * 128], qr[0:64, gi * N:gi * N + N], False, True, [kr_b, qr_b], [psb[sb]])
                    ACT(pT[sb][:, 0:N], ps[sb][:, 0:N], AF.Exp, [], [psb[sb], pT_b[sb]], scale=scale)
                l0_scores(0)
                for kt in range(NKT):
                    sb = kt % 2
                    if kt + 1 < NKT:
                        l0_scores(kt + 1)
                    PE(ps[6][:, 0:N], ones_b, pT[sb][:, 0:N], kt == 0, kt == NKT - 1, [pT_b[sb], cbuf], [psb[6]])
                    PE(ps[7][:, 0:N], vt[:, kt, :], pT[sb][:, 0:N], kt == 0, kt == NKT - 1, [pT_b[sb], vt_b], [psb[7]])
                DVE('reciprocal', [], [psb[6], rden_b], out=rden[:, 0:N], in_=ps[6][:, 0:N])
                DVE('tensor_tensor', [rden_b], [psb[7], atth_b], out=atth[:, 0, gi * N:gi * N + N], in0=ps[7][:, 0:N], in1=rden[:, 0:N], op=ALU.mult)
            out_proj(w_o_a, h, 1, atth, [atth_b], tok0, T, 2, bsel, banks=(4, 5))

    def ffn_phase(layer):
        moe = (layer == 1)
        P.barrier()
        nslot[0] = MAXSLOT
        sc2 = Scr(O_HF, ARENA)
        hT = sc2.b(NCH * TT).rearrange("p (c t) -> p c t", c=NCH)
        hT_b = [[Buf() for _ in range(3)] for _ in range(NCH)]
        if moe:
            wr32 = sc2.f(NCH * NE).rearrange("p (k e) -> p k e", e=NE)
            wr_b = Buf()
            P.dma('sp', wr32, w_router.rearrange("(k p) e -> p k e", p=128), msem(), writes=[wr_b])
            dg = sc2.f(12 * NE).rearrange("p (t e) -> p t e", e=NE)
            dg_b = Buf()
            mx8 = sc2.f(8)
            mx_b = Buf()
            lg = sc2.f(NE)
            lg_b = Buf()
            den1 = sc2.f(1)
            rep = sc2.f(128)
            rep_b = Buf()
        mark = sc2.p
        for (tok0, T, bsel) in ((0, 512, 0), (512, 1024, 1)):
            scm = Scr(mark, SCR_END)
            sub_hT = hT[:, :, tok0:tok0 + T]
            sub_b = [[hT_b[c][grp_of(tok0) + gi] for gi in range(T // 512)] for c in range(NCH)]
            if moe:
                bank = 5 if tok0 == 0 else 4
                modulate(sub_hT, sub_b, tok0, T, 3, 4, bsel, scm, side=(wr32, NE, bank, wr_b))
                for tl in range(T // 128):
                    tile = tok0 // 128 + tl
                    lgv = ps[bank][:, tl * NE:(tl + 1) * NE]
                    DVE('tensor_copy', [], [psb[bank], lg_b], out=lg, in_=lgv)
                    if 'lgt' in dbg_out:
                        P.dma('sp', dbg_out['lgt'][:, tile * NE:(tile + 1) * NE], lg, msem(), reads=[lg_b], is_output=True)
                    DVE('max', [lg_b], [mx_b], out=mx8, in_=lg)
                    DVE('tensor_scalar', [mx_b], [lg_b, dg_b], out=dg[:, tile, :], in0=lg, scalar1=mx8[:, 1:2], scalar2=None, op0=ALU.is_ge)
                    DVE('tensor_scalar', [mx_b], [lg_b], out=lg, in0=lg, scalar1=mx8[:, 0:1], scalar2=None, op0=ALU.subtract)
                    ACT(lg, lg, AF.Exp, [], [lg_b])
                    DVE('tensor_tensor', [lg_b], [dg_b], out=dg[:, tile, :], in0=dg[:, tile, :], in1=lg, op=ALU.mult)
                    DVE('reduce_sum', [dg_b], [mx_b], out=den1, in_=dg[:, tile, :], axis=AX.X)
                    DVE('reciprocal', [], [mx_b], out=den1, in_=den1)
                    DVE('tensor_scalar', [mx_b], [dg_b], out=dg[:, tile, :], in0=dg[:, tile, :], scalar1=den1[:, 0:1], scalar2=None, op0=ALU.mult)
            else:
                modulate(sub_hT, sub_b, tok0, T, 3, 4, bsel, scm)
        if moe and 'dg' in dbg_out:
            P.dma('sp', dbg_out['dg'], dg.rearrange("p t e -> p (t e)"), msem(), reads=[dg_b], is_output=True)
        P.barrier()
        sc3 = Scr(mark, SCR_END)
        sg = [sc3.f(512) for _ in range(2)]
        sg_b = [Buf() for _ in range(2)]
        actT = [[sc3.b(TT) for _ in range(2)] for _ in range(2)]
        actT_b = [[Buf() for _ in range(2)] for _ in range(2)]
        if moe:
            dgT = sc3.b(TT)
            dgT_b = Buf()
        nexp = ne_run if moe else 1
        assert nfc_run % 2 == 0
        prev = [None]

        def down_groups(lo, hi):
            if prev[0] is None:
                return
            sds, a = prev[0]
            for idx in range(lo, hi):
                m, g = divmod(idx, 3)
                bk = 4 + idx % 4
                for j in range(2):
                    sdv, sdb = sds[j]
                    PE(ps[bk], sdv[:, m * 128:(m + 1) * 128], actT[a][j][:, g * 512:(g + 1) * 512], j == 0, j == 1, [sdb, actT_b[a][j]], [psb[bk]])
                resid_add(bk, 0, 512, m, g * 512, 5, 0 if g == 0 else 1)

        pi = 0
        for e in range(nexp):
            if moe:
                for tile in range(12):
                    DVE('tensor_scalar', [dg_b, cbuf], [rep_b], out=rep, in0=ones_f, scalar1=dg[:, tile, e:e + 1], scalar2=None, op0=ALU.mult)
                    bk = 4 + tile % 4
                    PE(ps[bk][:, 0:128], rep, ident_f, True, True, [rep_b, cbuf], [psb[bk]])
                    P.op('act', lambda en, o=dgT[:, tile * 128:(tile + 1) * 128], i=ps[bk][:, 0:128]: en.copy(out=o, in_=i), [], [psb[bk], dgT_b])
                wg_, wu_, wd_ = w_eg[e], w_eu[e], w_ed[e]
            else:
                wg_, wu_, wd_ = w_fg, w_fu, w_fd
            for p in range(nfc_run // 2):
                a = pi % 2
                pi += 1
                gus = []
                for j in range(2):
                    f = 2 * p + j
                    gus.append((kslab(wg_, f * 128, 128), kslab(wu_, f * 128, 128)))
                sds = [slab(lambda r: r, wd_[(2 * p + j) * 128:(2 * p + j + 1) * 128, :]) for j in range(2)]
                blk = 0
                for j in range(2):
                    (sgv, sgb), (suv, sub) = gus[j]
                    for g in range(3):
                        bg, bu = (0, 1) if blk % 2 == 0 else (2, 3)
                        for k in range(NCH):
                            PE(ps[bg], sgv[:, k, :], hT[:, k, g * 512:(g + 1) * 512], k == 0, k == NCH - 1, [sgb, hT_b[k][g]], [psb[bg]])
                            if k % 4 == 3:
                                down_groups(blk * 8 + k // 4, blk * 8 + k // 4 + 1)
                        for k in range(NCH):
                            PE(ps[bu], suv[:, k, :], hT[:, k, g * 512:(g + 1) * 512], k == 0, k == NCH - 1, [sub, hT_b[k][g]], [psb[bu]])
                            if k % 4 == 3:
                                down_groups(blk * 8 + 4 + k // 4, blk * 8 + 4 + k // 4 + 1)
                        q = blk % 2
                        ACT(sg[q], ps[bg], AF.Silu, [], [psb[bg], sg_b[q]])
                        if moe:
                            DVE('tensor_tensor', [dgT_b], [sg_b[q]], out=sg[q], in0=sg[q], in1=dgT[:, g * 512:(g + 1) * 512], op=ALU.mult)
                        DVE('tensor_tensor', [sg_b[q]], [psb[bu], actT_b[a][j]], out=actT[a][j][:, g * 512:(g + 1) * 512], in0=ps[bu], in1=sg[q], op=ALU.mult)
                        blk += 1
                prev[0] = (sds, a)
        down_groups(0, 48)
        nslot[0] = 6

    def final_phase():
        P.barrier()
        sc = Scr(O_H, ARENA)
        stg = [sc.f(2048) for _ in range(2)]
        stg_b = [Buf() for _ in range(2)]
        stg_s = [P.dsem() for _ in range(2)]
        sq = [sc.b(512) for _ in range(2)]
        sq_b = [Buf() for _ in range(2)]
        rstd = sc.f(512)
        rstd_b = Buf()
        tmp = [sc.f(512) for _ in range(2)]
        tmp_b = [Buf() for _ in range(2)]
        tmpr = (sc.f(128), Buf())
        gfb = load_cols(gv[:, 2, :], g_final.rearrange("(f p) -> f p", p=128), 16, tmpr)
        for g in range(3):
            for c in range(NCH):
                q = c % 2
                ACT(sq[q], xT[:, c, g * 512:(g + 1) * 512], AF.Square, [xT_b[c][g]], [sq_b[q]])
                PE(ps[7], ones_b, sq[q], c == 0, c == NCH - 1, [sq_b[q], cbuf], [psb[7]])
            DVE('tensor_scalar', [], [psb[7], rstd_b], out=rstd, in0=ps[7], scalar1=1.0 / D, scalar2=EPS, op0=ALU.mult, op1=ALU.add)
            P.op('act', lambda e, o=rstd, i=rstd: e.sqrt(out=o, in_=i), [], [rstd_b])
            DVE('reciprocal', [], [rstd_b], out=rstd, in_=rstd)
            for c in range(NCH):
                q = c % 2
                DVE('scalar_tensor_tensor', [xT_b[c][g], rstd_b, gfb], [tmp_b[q]], out=tmp[q], in0=xT[:, c, g * 512:(g + 1) * 512],
                    scalar=gv[:, 2, c:c + 1], in1=rstd, op0=ALU.mult, op1=ALU.mult)
                bk = c % 4
                for tl in range(4):
                    TR(ps[bk][:, tl * 128:(tl + 1) * 128], tmp[q][:, tl * 128:(tl + 1) * 128], ident_f, [tmp_b[q], cbuf], [psb[bk]])
                s = c % 2
                if c % 2 == 0:
                    DVE('tensor_copy', [], [psb[bk], stg_b[s]], out=stg[s][:, 0:512], in_=ps[bk])
                else:
                    P.op('act', lambda e, o=stg[s][:, 0:512], i=ps[bk]: e.copy(out=o, in_=i), [], [psb[bk], stg_b[s]])
                dst = yout[g * 512:(g + 1) * 512, c * 128:(c + 1) * 128].rearrange("(j p) f -> p j f", p=128)
                P.dma('sp', dst, stg[s][:, 0:512].rearrange("p (j f) -> p j f", j=4), stg_s[s], reads=[stg_b[s]], is_output=True)


    LN16 = math.log(16.0)

    def l1_mixer(tok0, T, sample, seq):
        P.barrier()
        bsel = 1 if sample else 0
        N = min(512, T)
        NG = T // N
        NT = T // 128
        QN = 256
        NQ = T // QN
        nslot[0] = 8
        bigsel = [0]
        R = Scr(O_RING + 8 * 4096, ARENA)
        hT = R.b(NCH * T).rearrange("p (c t) -> p c t", c=NCH)
        hT_b = [[Buf() for _ in range(NG)] for _ in range(NCH)]
        gt = R.ft(NT * 16).rearrange("p (t g) -> p t g", g=16)
        lf8 = R.ft(NT * 8).rearrange("p (t g) -> p t g", g=8)
        Btm = R.ft(NT * 8).rearrange("p (t g) -> p t g", g=8)
        u0 = R.ft(NT * 8).rearrange("p (t g) -> p t g", g=8)
        uu = R.ft(NT * 8).rearrange("p (t g) -> p t g", g=8)
        wint = R.ft(NT * 8).rearrange("p (t g) -> p t g", g=8)
        wgt = R.ft(NT * 8).rearrange("p (t g) -> p t g", g=8)
        wg32 = R.ft(16 * 16).rearrange("p (k g) -> p k g", g=16)
        bg = R.ft(16)
        m0b = R.ft(8)
        ghb = R.ft(512)
        sm = R.ft(16)
        g_b, wg_b, bg_b, m0_b, gh_b, sm_b = Buf(), Buf(), Buf(), Buf(), Buf(), Buf()
        P.dma('sp', wg32, w_in_c[:, 6144:6160].rearrange("(k p) g -> p k g", p=128), msem(), writes=[wg_b])
        P.dma('sp', bg, b_gates[0:1, :].to_broadcast([128, 16]), msem(), writes=[bg_b])
        if sample:
            P.dma('sp', m0b, st_m[0:1, :].to_broadcast([128, 8]), msem(), writes=[m0_b])
        mark = R.p
        HI = R.hi
        scm = Scr(mark, HI)
        modulate(hT, hT_b, tok0, T, 0, 1, bsel, scm, side=(wg32, 16, 5, wg_b))
        P.barrier()
        DVE('tensor_tensor', [bg_b], [psb[5], g_b], out=gt, in0=ps[5][:, 0:NT * 16].rearrange("p (t g) -> p t g", g=16),
            in1=bg.unsqueeze(1).to_broadcast([128, NT, 16]), op=ALU.add)
        for d, c0 in ((0, 4), (1, 12)):
            v = gt[:, :, c0:c0 + 4]
            o = lf8[:, :, d * 4:d * 4 + 4]
            ACT(o, v, AF.Exp, [g_b], [sm_b], scale=-1.0)
            DVE('tensor_scalar', [], [sm_b], out=o, in0=o, scalar1=1.0, scalar2=None, op0=ALU.add)
            ACT(o, o, AF.Ln, [], [sm_b])
            DVE('tensor_scalar', [], [sm_b], out=o, in0=o, scalar1=-1.0, scalar2=None, op0=ALU.mult)
        for j in range(NT):
            seqs = [(i, ones_f) for i in range(j)] + [(j, triU)]
            for n_, (i, L) in enumerate(seqs):
                PE(ps[6][:, j * 8:j * 8 + 4], L, lf8[:, i, 0:4], n_ == 0, n_ == len(seqs) - 1, [sm_b, cbuf], [psb[6]])
            seqs = [(j, triL)] + [(i, ones_f) for i in range(j + 1, NT)]
            for n_, (i, L) in enumerate(seqs):
                PE(ps[6][:, j * 8 + 4:j * 8 + 8], L, lf8[:, i, 4:8], n_ == 0, n_ == len(seqs) - 1, [sm_b, cbuf], [psb[6]])
        B_b = Buf()
        DVE('tensor_copy', [], [psb[6], B_b], out=Btm, in_=ps[6][:, 0:NT * 8].rearrange("p (t g) -> p t g", g=8))
        u_b = Buf()
        DVE('tensor_tensor', [g_b, B_b], [u_b], out=u0[:, :, 0:4], in0=gt[:, :, 0:4], in1=Btm[:, :, 0:4], op=ALU.subtract)
        DVE('tensor_tensor', [g_b, B_b], [u_b], out=u0[:, :, 4:8], in0=gt[:, :, 8:12], in1=Btm[:, :, 4:8], op=ALU.subtract)
        DVE('tensor_scalar', [], [u_b], out=uu, in0=u0, scalar1=-LN16, scalar2=None, op0=ALU.add)
        if sample:
            DVE('tensor_tensor', [B_b, m0_b], [u_b], out=wint, in0=Btm, in1=m0b.unsqueeze(1).to_broadcast([128, NT, 8]), op=ALU.add)
            ACT(wint, wint, AF.Exp, [], [u_b])
        else:
            for j in range(NT):
                TR(ps[7][0:8, j * 128:(j + 1) * 128], u0[:, j, :], ident_f, [u_b, cbuf], [psb[7]])
            mx = sm[0:8, 0:1]
            mm = sm[0:8, 1:2]
            mv = sm[0:8, 2:3]
            dg8 = sm[0:8, 8:16]
            DVE('reduce_max', [], [psb[7], sm_b], out=mx, in_=ps[7][0:8, 0:T], axis=AX.X)
            DVE('tensor_scalar', [], [sm_b], out=mm, in0=mx, scalar1=0.0, scalar2=None, op0=ALU.max)
            for j in range(NT):
                PE(ps[6][0:8, 64:65], lf8[:, j, :], ones_f[:, 0:1], j == 0, j == NT - 1, [sm_b, cbuf], [psb[6]])
            DVE('tensor_tensor', [], [psb[6], sm_b], out=mv, in0=ps[6][0:8, 64:65], in1=mm, op=ALU.add)
            P.dma('sp', o_m[seq:seq + 1, :].rearrange("a d -> d a"), mv, msem(), reads=[sm_b], is_output=True)
            DVE('tensor_scalar', [cbuf], [sm_b], out=dg8, in0=ident_f[0:8, 0:8], scalar1=mm, scalar2=None, op0=ALU.mult)
            PE(ps[6][:, 72:80], ones_f[0:8, :], dg8, True, True, [sm_b, cbuf], [psb[6]])
            DVE('tensor_tensor', [u_b], [psb[6], u_b], out=wgt, in0=uu, in1=ps[6][:, 72:80].unsqueeze(1).to_broadcast([128, NT, 8]), op=ALU.subtract)
            ACT(wgt, wgt, AF.Exp, [], [u_b])

        for h in range(4):
            P.barrier()
            sc = Scr(mark, HI)
            qT = sc.b(2 * T).rearrange("p (k t) -> p k t", k=2)
            kT = sc.b(2 * T).rearrange("p (k t) -> p k t", k=2)
            vtm = sc.b(NT * 512).rearrange("p (t e) -> p t e", e=512)
            q_b, k_b, v_b = Buf(), Buf(), Buf()
            if not sample:
                ktm = sc.b(NT * 256).rearrange("p (t e) -> p t e", e=256)
                kw = sc.b(256)
                ktm_b, kw_b = Buf(), Buf()
                cst = [sc.f(512) for _ in range(2)]
                cst_b = [Buf() for _ in range(2)]
                cst_s = [P.dsem() for _ in range(2)]
                nst = sc.f(4)
                nst_b = Buf()
            else:
                C0 = sc.b(1024).rearrange("p (k e) -> p k e", k=2)
                n0 = sc.b(2)
                C0_b = Buf()
            sig = sc.b(2 * 512).rearrange("p (t e) -> p t e", e=512)
            sig_b = Buf()
            Bbc = sc.f(QN)
            Bbc_b = Buf()
            rep = sc.f(128)
            rep_b = Buf()
            hacc = sc.f(2 * 512).rearrange("p (t e) -> p t e", e=512)
            hacc_b = Buf()
            Ws = [sc.f(QN) for _ in range(2)]
            Ws_b = [Buf() for _ in range(2)]
            ST = [sc.b(QN) for _ in range(2)]
            ST_b = [Buf() for _ in range(2)]
            dtmps = [sc.f(128) for _ in range(2)]
            dtmps_b = [Buf() for _ in range(2)]
            t1 = sc.f(512)
            t1_b = Buf()
            ybf = sc.b(512)
            ybf_b = Buf()
            yT = sc.b(4 * QN).rearrange("p (k t) -> p k t", k=4)
            yT_b = Buf()
            rr = sc.f(8)
            rr_b = Buf()
            P.dma('sp', ghb, g_h[0:1, h * 512:(h + 1) * 512].to_broadcast([128, 512]), msem(), writes=[gh_b])
            qkrot = [0]
            for (dst, dstb, cbase) in ((qT, q_b, h * 256), (kT, k_b, 1024 + h * 256)):
                for kc in range(2):
                    for gi in range(NG):
                        wv, wb = kslab(w_in_c, cbase + kc * 128, 128)
                        qb_ = (0, 3, 4, 7)[qkrot[0] % 4]
                        qkrot[0] += 1
                        for k in range(NCH):
                            PE(ps[qb_][:, 0:N], wv[:, k, :], hT[:, k, gi * N:gi * N + N], k == 0, k == NCH - 1, [wb, hT_b[k][gi]], [psb[qb_]])
                        P.op('act', lambda e, o=dst[:, kc, gi * N:gi * N + N], i=ps[qb_][:, 0:N]: e.copy(out=o, in_=i), [], [psb[qb_], dstb])
            def big_slab(c0, ncols):
                ns = ncols // 128
                s0 = 4 * (bigsel[0] % 2)
                bigsel[0] += 1
                v = bview(O_RING + s0 * 4096, 2048 * ns).rearrange("p (k c) -> p k c", k=16)
                P.dma('pool', v, w_in_c[:, c0:c0 + ncols].rearrange("(k p) c -> p k c", p=128), ring_s[s0], writes=[ring_b[s0 + i_] for i_ in range(ns)])
                ring_i[0] = (s0 + 4) % 8
                return v, [ring_b[s0 + i_] for i_ in range(ns)]

            def tm_proj(c0, ncols, tiles, evac):
                wv, wbs = big_slab(c0, ncols)
                for jl, j in enumerate(tiles):
                    bk = 1 + jl % 2
                    for k in range(NCH):
                        PE(ps[bk][:, 0:ncols], hT[:, k, j * 128:(j + 1) * 128], wv[:, k, :], k == 0, k == NCH - 1,
                           wbs + [hT_b[k][(j * 128) // N]], [psb[bk]])
                    evac(jl, j, bk)
            tm_proj(2048 + h * 512, 512, range(NT), lambda jl, j, bk: P.op('act', lambda e, o=vtm[:, j, :], i=ps[bk]: e.copy(out=o, in_=i), [], [psb[bk], v_b]))
            if not sample:
                tm_proj(1024 + h * 256, 256, range(NT), lambda jl, j, bk: DVE('tensor_copy', [], [psb[bk], ktm_b], out=ktm[:, j, :], in_=ps[bk][:, 0:256]))
            for qg in range(NQ):
                jt0 = qg * 2
                tm_proj(4096 + h * 512, 512, [jt0, jt0 + 1],
                        lambda jl, j, bk: ACT(sig[:, jl, :], ps[bk], AF.Sigmoid, [], [psb[bk], sig_b]))
                for d in range(2):
                    col = d * 4 + h
                    for jl in range(2):
                        DVE('tensor_scalar', [B_b, cbuf], [rep_b], out=rep, in0=ones_f, scalar1=Btm[:, jt0 + jl, col:col + 1], scalar2=None, op0=ALU.mult)
                        PE(ps[7][:, jl * 128:(jl + 1) * 128], rep, ident_f, True, True, [rep_b, cbuf], [psb[7]])
                    DVE('tensor_copy', [], [psb[7], Bbc_b], out=Bbc, in_=ps[7][:, 0:QN])
                    if sample:
                        P.dma('pool', C0, st_C[d, h].rearrange("(k p) e -> p k e", p=128), pmsem(), writes=[C0_b])
                        P.dma('pool', n0.rearrange("p (k o) -> p k o", o=1), st_n[d, h].rearrange("(k p o) -> p k o", p=128, o=1), pmsem(), writes=[C0_b], allow_slow_non_contiguous=True)
                    sts = list(range(0, jt0 + 2)) if d == 0 else list(range(jt0, NT))
                    first_den = [True]
                    def vals(st):
                        if d == 0:
                            return [jl for jl in range(2) if jt0 + jl >= st]
                        return [jl for jl in range(2) if jt0 + jl <= st]

                    def l1_scores(si, st):
                        sb = 5 + si % 2
                        q = si % 2
                        W, W_b, dtmp, dtmp_b = Ws[q], Ws_b[q], dtmps[q], dtmps_b[q]
                        for kc in range(2):
                            PE(ps[sb][:, 0:QN], kT[:, kc, st * 128:(st + 1) * 128], qT[:, kc, qg * QN:(qg + 1) * QN], kc == 0, kc == 1, [k_b, q_b], [psb[sb]])
                        val = vals(st)
                        for jl in val:
                            cs = slice(jl * 128, (jl + 1) * 128)
                            if jt0 + jl == st:
                                DVE('tensor_tensor', [Bbc_b, cbuf], [dtmp_b], out=dtmp, in0=Bbc[:, cs], in1=(negU if d == 0 else negL), op=ALU.add)
                                ACT(W[:, cs], dtmp, AF.Exp, [dtmp_b, u_b], [W_b], bias=uu[:, st, col:col + 1], scale=1.0)
                            else:
                                ACT(W[:, cs], Bbc[:, cs], AF.Exp, [Bbc_b, u_b], [W_b], bias=uu[:, st, col:col + 1], scale=1.0)
                        c0, c1 = val[0] * 128, (val[-1] + 1) * 128
                        DVE('tensor_tensor', [W_b], [psb[sb], ST_b[q]], out=ST[q][:, c0:c1], in0=ps[sb][:, c0:c1], in1=W[:, c0:c1], op=ALU.mult)

                    l1_scores(0, sts[0])
                    for si, st in enumerate(sts):
                        q = si % 2
                        if si + 1 < len(sts):
                            l1_scores(si + 1, sts[si + 1])
                        for jl in vals(st):
                            jt = jt0 + jl
                            if d == 0:
                                fst, lst = (st == 0), (st == jt)
                            else:
                                fst, lst = (st == jt), (st == NT - 1)
                            PE(ps[jl], ST[q][:, jl * 128:(jl + 1) * 128], vtm[:, st, :], fst, lst, [ST_b[q], v_b], [psb[jl]])
                            PE(ps[4][:, jl:jl + 1], ST[q][:, jl * 128:(jl + 1) * 128], ones_b[:, 0:1], first_den[0], lst, [ST_b[q], cbuf], [psb[4]],
                               skip_group_check=True)
                            first_den[0] = False
                    for jl in range(2):
                        jt = jt0 + jl
                        den = rr[:, 0:1]
                        wr_ = rr[:, 1:2]
                        nbk = 3 if jl == 0 else 7
                        if sample:
                            for kc in range(2):
                                PE(ps[nbk], qT[:, kc, jt * 128:(jt + 1) * 128], C0[:, kc, :], kc == 0, kc == 1, [q_b, C0_b], [psb[nbk]])
                            for kc in range(2):
                                PE(ps[6][:, 0:1], qT[:, kc, jt * 128:(jt + 1) * 128], n0[:, kc:kc + 1], kc == 0, kc == 1, [q_b, C0_b], [psb[6]])
                            DVE('tensor_copy', [], [psb[6], rr_b], out=wr_, in_=ps[6][:, 0:1])
                            DVE('scalar_tensor_tensor', [u_b], [psb[4], rr_b], out=den, in0=wr_, scalar=wint[:, jt, col:col + 1], in1=ps[4][:, jl:jl + 1],
                                op0=ALU.mult, op1=ALU.add)
                        else:
                            DVE('tensor_copy', [], [psb[4], rr_b], out=den, in_=ps[4][:, jl:jl + 1])
                        DVE('tensor_scalar', [], [rr_b], out=rr[:, 3:4], in0=den, scalar1=-1.0, scalar2=None, op0=ALU.mult)
                        DVE('tensor_tensor', [], [rr_b], out=den, in0=den, in1=rr[:, 3:4], op=ALU.max)
                        DVE('tensor_scalar', [], [rr_b], out=den, in0=den, scalar1=1.0, scalar2=None, op0=ALU.max)
                        DVE('reciprocal', [], [rr_b], out=den, in_=den)
                        if d == 0:
                            DVE('tensor_scalar', [rr_b], [psb[jl], hacc_b], out=hacc[:, jl, :], in0=ps[jl], scalar1=den, scalar2=None, op0=ALU.mult)
                        else:
                            DVE('scalar_tensor_tensor', [rr_b], [psb[jl], hacc_b], out=hacc[:, jl, :], in0=ps[jl], scalar=den, in1=hacc[:, jl, :],
                                op0=ALU.mult, op1=ALU.add)
                        if sample:
                            DVE('tensor_tensor', [u_b], [rr_b], out=wr_, in0=den, in1=wint[:, jt, col:col + 1], op=ALU.mult)
                            DVE('scalar_tensor_tensor', [rr_b], [psb[nbk], hacc_b], out=hacc[:, jl, :], in0=ps[nbk], scalar=wr_, in1=hacc[:, jl, :],
                                op0=ALU.mult, op1=ALU.add)
                for jl in range(2):
                    ssq = rr[:, 2:3]
                    ACT(t1, hacc[:, jl, :], AF.Square, [hacc_b], [t1_b, rr_b], accum_out=ssq)
                    DVE('tensor_scalar', [], [rr_b], out=ssq, in0=ssq, scalar1=1.0 / 512, scalar2=EPS, op0=ALU.mult, op1=ALU.add)
                    P.op('act', lambda e, o=ssq, i=ssq: e.sqrt(out=o, in_=i), [], [rr_b])
                    DVE('reciprocal', [], [rr_b], out=ssq, in_=ssq)
                    DVE('scalar_tensor_tensor', [hacc_b, rr_b, gh_b], [t1_b], out=t1, in0=hacc[:, jl, :], scalar=ssq, in1=ghb, op0=ALU.mult, op1=ALU.mult)
                    DVE('tensor_tensor', [sig_b], [t1_b, ybf_b], out=ybf, in0=t1, in1=sig[:, jl, :], op=ALU.mult)
                    pb = ps[7].bitcast(BF16)
                    for ec in range(4):
                        TR(pb[:, ec * 128:(ec + 1) * 128], ybf[:, ec * 128:(ec + 1) * 128], ident_b, [ybf_b, cbuf], [psb[7]])
                    DVE('tensor_copy', [], [psb[7], yT_b], out=yT[:, :, jl * 128:(jl + 1) * 128], in_=pb[:, 0:512].rearrange("p (k t) -> p k t", k=4))
                out_proj(w_o_c, 4 * h, 4, yT, [yT_b], tok0 + qg * QN, QN, 2, bsel, banks=(5, 6))
            if not sample:
                oi = 0
                for d in range(2):
                    col = d * 4 + h
                    for dc in range(2):
                        for j in range(NT):
                            DVE('tensor_scalar', [ktm_b, u_b], [kw_b], out=kw[:, 0:128], in0=ktm[:, j, dc * 128:(dc + 1) * 128],
                                scalar1=wgt[:, j, col:col + 1], scalar2=None, op0=ALU.mult)
                            PE(ps[0], kw[:, 0:128], vtm[:, j, :], j == 0, j == NT - 1, [kw_b, v_b], [psb[0]])
                            PE(ps[1][:, 0:1], kw[:, 0:128], ones_b[:, 0:1], j == 0, j == NT - 1, [kw_b, cbuf], [psb[1]])
                        s = oi % 2
                        oi += 1
                        DVE('tensor_copy', [], [psb[0], cst_b[s]], out=cst[s], in_=ps[0])
                        P.dma('sp', o_C[seq, d, h, dc * 128:(dc + 1) * 128, :], cst[s], cst_s[s], reads=[cst_b[s]], is_output=True)
                        DVE('tensor_copy', [], [psb[1], nst_b], out=nst[:, 0:1], in_=ps[1][:, 0:1])
                        P.dma('sp', o_n[seq, d, h, dc * 128:(dc + 1) * 128].rearrange("(p o) -> p o", o=1), nst[:, 0:1], msem(), reads=[nst_b], is_output=True)
        nslot[0] = 6

    def dump(name):
        P.barrier()
        bufs = [xT_b[c][g] for c in range(NCH) for g in range(3)]
        P.dma('sp', dbg_out[name], fview(O_X, NCH * TT), msem(), reads=bufs, is_output=True)

    load_x()
    if 'l0mix' in S or 'l0ffn' in S:
        adaln(0, True)
    if 'l0mix' in S:
        l0_mixer(0, TP, False, 0)
        l0_mixer(TP, TP, False, 1)
        l0_mixer(2 * TP, TS, True, 0)
        if 'x_mix0' in dbg_out:
            dump('x_mix0')
    if 'l0ffn' in S:
        ffn_phase(0)
        if 'x_ffn0' in dbg_out:
            dump('x_ffn0')
    if 'l1mix' in S or 'l1ffn' in S:
        adaln(1, not ('l0mix' in S or 'l0ffn' in S))
    if 'l1mix' in S:
        l1_mixer(0, TP, False, 0)
        l1_mixer(TP, TP, False, 1)
        l1_mixer(2 * TP, TS, True, 0)
        if 'x_mix1' in dbg_out:
            dump('x_mix1')
    if 'l1ffn' in S:
        ffn_phase(1)
        if 'x_ffn1' in dbg_out:
            dump('x_ffn1')
    if 'final' in S:
        final_phase()
    stats = P.finalize()
    return nc, stats


def rope_tables():
    T = TS
    rows = T // 64
    row = np.repeat(np.arange(rows, dtype=np.float32), 64)
    col = np.tile(np.arange(64, dtype=np.float32), rows)
    half = 32
    inv = (10000.0 ** (-np.arange(0, half, 2, dtype=np.float32) / half)).astype(np.float32)
    ar = row[:, None] * inv
    ac = col[:, None] * inv
    cosT = np.zeros((64, T), np.float32)
    sinT = np.zeros((64, T), np.float32)
    cosT[0:16] = np.cos(ar).T
    cosT[16:32] = np.cos(ar).T
    cosT[32:48] = np.cos(ac).T
    cosT[48:64] = np.cos(ac).T
    sinT[0:16] = -np.sin(ar).T
    sinT[16:32] = np.sin(ar).T
    sinT[32:48] = -np.sin(ac).T
    sinT[48:64] = np.sin(ac).T
    return cosT, sinT


_CACHE = {}


def kernel(x_prompt, x_sample, c, cache_ckv, cache_krope, state_C, state_n, state_m,
           c_ctx, w_mod, b_mod, g_mix, g_ffn,
           w_in_a, g_q, g_kv, w_uq, w_ukv, conv_w, w_o_a,
           w_in_c, b_gates, g_h, w_o_c,
           w_ffn_gate, w_ffn_up, w_ffn_down,
           w_router, w_exp_gate, w_exp_up, w_exp_down, g_final):
    f = lambda a: np.ascontiguousarray(np.asarray(a), dtype=np.float32)
    ncores = 8
    if 'nc' not in _CACHE:
        _CACHE['nc'] = build()[0]
    nc = _CACHE['nc']
    cosT, sinT = rope_tables()
    shared = dict(
        w_mod=f(w_mod), b_mod=f(b_mod), g_mix=f(g_mix), g_ffn=f(g_ffn),
        w_in_a=f(w_in_a[0]), g_q=f(g_q[0]), g_kv=f(g_kv[0]), w_uq=f(w_uq[0]), w_ukv=f(w_ukv[0]), conv_w=f(conv_w[0]), w_o_a=f(w_o_a[0]),
        ropec=cosT, ropes=sinT,
        w_fg=f(w_ffn_gate[0]), w_fu=f(w_ffn_up[0]), w_fd=f(w_ffn_down[0]),
        w_in_c=f(w_in_c[0]), b_gates=f(b_gates).reshape(1, 16), g_h=f(g_h).reshape(1, D), w_o_c=f(w_o_c[0]),
        w_router=f(w_router[0]), w_eg=f(w_exp_gate[0]), w_eu=f(w_exp_up[0]), w_ed=f(w_exp_down[0]), g_final=f(g_final),
    )
    x_prompt = np.asarray(x_prompt)
    x_sample = np.asarray(x_sample)
    in_maps = []
    for i in range(ncores):
        m = dict(shared)
        m["xin"] = f(np.concatenate([x_prompt[2 * i:2 * i + 2].reshape(2 * TP, D), x_sample[i]], 0))
        m["cvec"] = f(np.stack([np.asarray(c_ctx), np.asarray(c)[i]], 0))
        m["cache_ckv"] = f(np.asarray(cache_ckv)[i, 0])
        m["cache_kr"] = f(np.asarray(cache_krope)[i, 0])
        m["st_C"] = f(np.asarray(state_C)[i, 0])
        m["st_n"] = f(np.asarray(state_n)[i, 0])
        m["st_m"] = f(np.asarray(state_m)[i, 0]).reshape(1, 8)
        in_maps.append(m)
    res = run_bass_kernel_spmd(nc, in_maps, core_ids=list(range(ncores)))
    R = res.results
    y_prompt = np.zeros((16, TP, D), np.float32)
    y_sample = np.zeros((8, TS, D), np.float32)
    n_ckv = np.zeros((16, 1, TP, 512), np.float32)
    n_kr = np.zeros((16, 1, TP, 64), np.float32)
    n_C = np.zeros((16, 1, 2, 4, 256, 512), np.float32)
    n_n = np.zeros((16, 1, 2, 4, 256), np.float32)
    n_m = np.zeros((16, 1, 2, 4), np.float32)
    for i in range(ncores):
        r = R[i]
        y_prompt[2 * i:2 * i + 2] = r["yout"][:2 * TP].reshape(2, TP, D)
        y_sample[i] = r["yout"][2 * TP:]
        n_ckv[2 * i:2 * i + 2, 0] = r["o_ckv"]
        n_kr[2 * i:2 * i + 2, 0] = r["o_kr"]
        n_C[2 * i:2 * i + 2, 0] = r["o_C"]
        n_n[2 * i:2 * i + 2, 0] = r["o_n"]
        n_m[2 * i:2 * i + 2, 0] = r["o_m"].reshape(2, 2, 4)
    return (y_prompt, y_sample, n_ckv, n_kr, n_C, n_n, n_m)
```
